# Optimizing a Trainium2 kernel written in Bass

```python
import jax, jax.numpy as jnp
from jax import lax
import numpy as np

D_MODEL = 1024
BATCH = 16
SEQ = 4096
DEPTH = 4

N_A = DEPTH // 2
N_B = DEPTH - N_A
N_HEADS = 8
HEAD_DIM = D_MODEL // (2 * N_HEADS)
V_DIM = 2 * HEAD_DIM
CONV_WIDTH = 3
N_GROUPS = 4
EXPERTS_PER_GROUP = 4
N_EXPERTS = N_GROUPS * EXPERTS_PER_GROUP
TOP_K = 2
D_EXPERT = D_MODEL // 2
Q_BLOCK = 128
EPS = 1e-6
NEG_INF = -1e30

kernel_name = "yoco_shortconv_diffattn_hmoe_adaln"


def rmsnorm(x, g):
    xf = x.astype(jnp.float32)
    y = xf * lax.rsqrt(jnp.mean(xf * xf, axis=-1, keepdims=True) + EPS)
    return (y * g.astype(jnp.float32)).astype(x.dtype)


def modulate(h, shift, scale):
    return h * (1 + scale[:, None, :]) + shift[:, None, :]


def alibi_slopes(n_heads):
    return jnp.exp2(-8.0 * (jnp.arange(n_heads, dtype=jnp.float32) + 1.0) / n_heads)


def short_conv_mixer(h, w_in, conv_w, conv_b, w_out):
    u = h @ w_in
    b_gate, c_gate, v = jnp.split(u, 3, axis=-1)
    z = c_gate * v
    kern = conv_w[:, None, :]
    zc = lax.conv_general_dilated(
        z, kern.astype(z.dtype), window_strides=(1,),
        padding=((CONV_WIDTH - 1, 0),),
        dimension_numbers=('NWC', 'WIO', 'NWC'),
        feature_group_count=D_MODEL) + conv_b
    return (b_gate * zc) @ w_out


def shared_kv(x, c, kv_mod_w, kv_mod_b, kv_norm_g, kv_w):
    bsz, seq, _ = x.shape
    m = jax.nn.silu(c) @ kv_mod_w + kv_mod_b
    shift, scale = jnp.split(m, 2, axis=-1)
    h = modulate(rmsnorm(x, kv_norm_g), shift, scale)
    kv = h @ kv_w
    k, v = jnp.split(kv, 2, axis=-1)
    k = k.reshape(bsz, seq, N_HEADS, 2, HEAD_DIM).transpose(3, 0, 2, 1, 4)
    v = v.reshape(bsz, seq, N_HEADS, V_DIM).transpose(0, 2, 1, 3)
    return k[0], k[1], v


def diff_attention(h, k1, k2, v, q_w, lq1, lk1, lq2, lk2, subln_g, o_w, lambda_init):
    bsz, seq, _ = h.shape
    nb = seq // Q_BLOCK
    q = (h @ q_w).reshape(bsz, nb, Q_BLOCK, N_HEADS, 2, HEAD_DIM)
    q = q.transpose(1, 4, 0, 3, 2, 5)
    lam = (jnp.exp(jnp.sum(lq1.astype(jnp.float32) * lk1.astype(jnp.float32)))
           - jnp.exp(jnp.sum(lq2.astype(jnp.float32) * lk2.astype(jnp.float32)))
           + lambda_init)
    slopes = alibi_slopes(N_HEADS)
    kpos = jnp.arange(seq)
    scale = HEAD_DIM ** -0.5

    def block(args):
        qb, start = args
        qpos = start + jnp.arange(Q_BLOCK)
        dist = qpos[:, None] - kpos[None, :]
        causal = dist >= 0
        bias = -slopes[:, None, None] * dist.astype(jnp.float32)[None]
        s1 = jnp.einsum('bhqd,bhkd->bhqk', qb[0], k1).astype(jnp.float32) * scale + bias
        s2 = jnp.einsum('bhqd,bhkd->bhqk', qb[1], k2).astype(jnp.float32) * scale + bias
        s1 = jnp.where(causal, s1, NEG_INF)
        s2 = jnp.where(causal, s2, NEG_INF)
        a = jax.nn.softmax(s1, axis=-1) - lam * jax.nn.softmax(s2, axis=-1)
        return jnp.einsum('bhqk,bhkv->bhqv', a.astype(v.dtype), v)

    starts = jnp.arange(nb) * Q_BLOCK
    o = lax.map(block, (q, starts))
    o = o.transpose(1, 0, 3, 2, 4).reshape(bsz, seq, N_HEADS, V_DIM)
    o = rmsnorm(o, subln_g) * (1.0 - lambda_init)
    return o.reshape(bsz, seq, N_HEADS * V_DIM) @ o_w


def hier_moe(h, gw, gb, ew, eb, w1, w3, w2):
    bsz, seq, d = h.shape
    t = h.reshape(-1, d)
    g_logits = (t @ gw + gb).astype(jnp.float32)
    p_g = jax.nn.softmax(g_logits, axis=-1)
    g_sel = jnp.argmax(g_logits, axis=-1)
    pg_sel = jnp.take_along_axis(p_g, g_sel[:, None], axis=-1)[:, 0]
    e_logits = (t @ ew + eb).astype(jnp.float32).reshape(-1, N_GROUPS, EXPERTS_PER_GROUP)
    e_sel_logits = jnp.take_along_axis(e_logits, g_sel[:, None, None], axis=1)[:, 0]
    p_e = jax.nn.softmax(e_sel_logits, axis=-1)
    top_v, top_i = lax.top_k(p_e, TOP_K)
    top_v = top_v / jnp.sum(top_v, axis=-1, keepdims=True)
    w_exp = jnp.sum(jax.nn.one_hot(top_i, EXPERTS_PER_GROUP, dtype=jnp.float32) * top_v[..., None], axis=1)
    combine = (jax.nn.one_hot(g_sel, N_GROUPS, dtype=jnp.float32)[:, :, None]
               * w_exp[:, None, :] * pg_sel[:, None, None]).reshape(-1, N_EXPERTS).astype(t.dtype)
    out = jnp.zeros_like(t)
    for e in range(N_EXPERTS):
        hid = jax.nn.silu(t @ w1[e]) * (t @ w3[e])
        out = out + combine[:, e:e + 1] * (hid @ w2[e])
    return out.reshape(bsz, seq, d)


def setup_inputs(seed: int = 0) -> dict:
    key = jax.random.key(seed)
    ks = iter(jax.random.split(key, 40))
    D = D_MODEL
    f32 = jnp.float32

    def nrm(shape, s):
        return jax.random.normal(next(ks), shape, f32) * s

    return {
        "x": nrm((BATCH, SEQ, D), 1.0),
        "c": nrm((BATCH, D), 1.0),
        "mod_w": nrm((DEPTH, D, 6 * D), 0.5 * D ** -0.5),
        "mod_b": nrm((DEPTH, 6 * D), 0.02),
        "norm_mix_g": 1.0 + nrm((DEPTH, D), 0.02),
        "norm_ffn_g": 1.0 + nrm((DEPTH, D), 0.02),
        "conv_in_w": nrm((N_A, D, 3 * D), D ** -0.5),
        "conv_w": nrm((N_A, CONV_WIDTH, D), CONV_WIDTH ** -0.5),
        "conv_b": nrm((N_A, D), 0.02),
        "conv_out_w": nrm((N_A, D, D), D ** -0.5),
        "kv_mod_w": nrm((D, 2 * D), 0.5 * D ** -0.5),
        "kv_mod_b": nrm((2 * D,), 0.02),
        "kv_norm_g": 1.0 + nrm((D,), 0.02),
        "kv_w": nrm((D, 2 * D), D ** -0.5),
        "q_w": nrm((N_B, D, D), D ** -0.5),
        "lam_q1": nrm((N_B, HEAD_DIM), 0.1),
        "lam_k1": nrm((N_B, HEAD_DIM), 0.1),
        "lam_q2": nrm((N_B, HEAD_DIM), 0.1),
        "lam_k2": nrm((N_B, HEAD_DIM), 0.1),
        "subln_g": 1.0 + nrm((N_B, V_DIM), 0.02),
        "o_w": nrm((N_B, D, D), D ** -0.5),
        "router_group_w": nrm((DEPTH, D, N_GROUPS), D ** -0.5),
        "router_group_b": nrm((DEPTH, N_GROUPS), 0.01),
        "router_exp_w": nrm((DEPTH, D, N_EXPERTS), D ** -0.5),
        "router_exp_b": nrm((DEPTH, N_EXPERTS), 0.01),
        "exp_w1": nrm((DEPTH, N_EXPERTS, D, D_EXPERT), D ** -0.5),
        "exp_w3": nrm((DEPTH, N_EXPERTS, D, D_EXPERT), D ** -0.5),
        "exp_w2": nrm((DEPTH, N_EXPERTS, D_EXPERT, D), D_EXPERT ** -0.5),
        "final_norm_g": 1.0 + nrm((D,), 0.02),
    }


def reference(x, c, mod_w, mod_b, norm_mix_g, norm_ffn_g, conv_in_w, conv_w, conv_b,
              conv_out_w, kv_mod_w, kv_mod_b, kv_norm_g, kv_w, q_w, lam_q1, lam_k1,
              lam_q2, lam_k2, subln_g, o_w, router_group_w, router_group_b,
              router_exp_w, router_exp_b, exp_w1, exp_w3, exp_w2, final_norm_g):
    c_act = jax.nn.silu(c)
    k1 = k2 = v = None
    for l in range(DEPTH):
        m = c_act @ mod_w[l] + mod_b[l]
        sh_a, sc_a, gt_a, sh_f, sc_f, gt_f = jnp.split(m, 6, axis=-1)
        if l == N_A:
            k1, k2, v = shared_kv(x, c, kv_mod_w, kv_mod_b, kv_norm_g, kv_w)
        h = modulate(rmsnorm(x, norm_mix_g[l]), sh_a, sc_a)
        if l < N_A:
            y = short_conv_mixer(h, conv_in_w[l], conv_w[l], conv_b[l], conv_out_w[l])
        else:
            j = l - N_A
            lambda_init = 0.8 - 0.6 * float(np.exp(-0.3 * l))
            y = diff_attention(h, k1, k2, v, q_w[j], lam_q1[j], lam_k1[j], lam_q2[j],
                               lam_k2[j], subln_g[j], o_w[j], lambda_init)
        x = x + gt_a[:, None, :] * y
        h = modulate(rmsnorm(x, norm_ffn_g[l]), sh_f, sc_f)
        y = hier_moe(h, router_group_w[l], router_group_b[l], router_exp_w[l],
                     router_exp_b[l], exp_w1[l], exp_w3[l], exp_w2[l])
        x = x + gt_f[:, None, :] * y
    return rmsnorm(x, final_norm_g)
```

```python
import numpy as np
import concourse.bass as bass
import concourse.mybir as mybir
from concourse.bass_utils import run_bass_kernel_spmd

F32 = mybir.dt.float32
BF16 = mybir.dt.bfloat16
AF = mybir.ActivationFunctionType
ALU = mybir.AluOpType
AX = mybir.AxisListType

D = 1024
NCH = 8
SEQ = 4096
NB_CORE = 2
TOK = NB_CORE * SEQ
TS = 1024
NST = TOK // TS
DEPTH = 4
N_A = 2
NH = 8
NE = 16
DE = 512
EPS = 1e-6
SLOPES = [2.0 ** (-8.0 * (h + 1) / NH) for h in range(NH)]

V_MODB = 0
V_KVB = V_MODB + 4 * 48
V_GMIX = V_KVB + 16
V_GFFN = V_GMIX + 32
V_GKV = V_GFFN + 32
V_GFIN = V_GKV + 8
V_CW = V_GFIN + 8
V_CB = V_CW + 48
NV = V_CB + 16

C_ID = 0
C_D = 128
C_DM = 128 + 512
NCONST = 128 + 1024


class Buf:
    __slots__ = ("name", "w", "r", "sem", "cnt")

    def __init__(self, name):
        self.name = name
        self.w = None
        self.r = {}
        self.sem = None
        self.cnt = 0


class Prog:
    ENGS = ("pe", "act", "dve", "pool", "sp")

    def __init__(self, nc):
        self.nc = nc
        self.ops = {e: [] for e in self.ENGS}
        self.semh = {}
        self.cnt = {}
        self.nsem = 0
        for e in ("pe", "act", "dve", "pool"):
            self._newsem(e)
        self.last = {}
        self.pend_bar = {e: [] for e in self.ENGS}
        self.dma_pending = []
        self.nops = 0

    def _alloc_sem(self, name):
        self.nsem += 1
        return self.nc.alloc_semaphore(f"s{self.nsem}_{name}")

    def _newsem(self, e):
        self.semh[e] = self._alloc_sem(e)
        self.cnt[e] = 0

    def _tick(self, e):
        if self.cnt[e] >= 30000:
            self._newsem(e)
        self.cnt[e] += 1
        tok = (self.semh[e], self.cnt[e], e)
        self.last[e] = tok
        return tok

    def _deps(self, reads, writes, gid=None):
        waits = []
        for b in reads:
            if b.w is not None:
                waits.append(b.w)
        for b in writes:
            if b.w is not None and not (gid is not None and b.w[2] == ("dma", gid)):
                waits.append(b.w)
            waits.extend(b.r.values())
        return waits

    def _record(self, tok, reads, writes):
        key = (tok[0].num, tok[2])
        for b in reads:
            b.r[key] = tok
        for b in writes:
            b.w = tok
            b.r = {}

    def op(self, eng, fn, reads=(), writes=()):
        waits = self._deps(reads, writes)
        if self.pend_bar[eng]:
            waits.extend(self.pend_bar[eng])
            self.pend_bar[eng] = []
        tok = self._tick(eng)
        self.ops[eng].append((waits, fn, tok, 1))
        self._record(tok, reads, writes)
        self.nops += 1
        return tok

    def dma(self, q, fn, reads=(), writes=(), gid=None, exempt=False):
        b0 = writes[0]
        waits = self._deps(reads, writes, gid)
        if self.pend_bar[q] and not exempt:
            waits.extend(self.pend_bar[q])
            self.pend_bar[q] = []
        if b0.sem is None or b0.cnt >= 30000:
            b0.sem = self._alloc_sem("d" + b0.name)
            b0.cnt = 0
        b0.cnt += 16
        tok = (b0.sem, b0.cnt, ("dma", gid))
        self.ops[q].append((waits, fn, tok, 16))
        self._record(tok, reads, writes)
        if not exempt:
            self.dma_pending.append(tok)
        self.nops += 1
        return tok

    def barrier(self):
        toks = [self.last[e] for e in ("pe", "act", "dve", "pool") if e in self.last]
        toks.extend(self.dma_pending)
        self.dma_pending = []
        for e in self.ENGS:
            self.pend_bar[e] = list(self.pend_bar[e]) + toks

    def emit(self, final_waits):
        nc = self.nc
        engmap = {"pe": "tensor", "act": "scalar", "dve": "vector", "pool": "gpsimd", "sp": "sync"}
        with nc.Block() as block:
            for e in self.ENGS:
                def body(eng, e=e):
                    waited = {}
                    for waits, fn, tok, inc in self.ops[e]:
                        need = {}
                        for (sh, v, pe) in waits:
                            if pe == "pe" and e == "pe":
                                continue
                            k = sh.num
                            if waited.get(k, 0) >= v:
                                continue
                            if k not in need or need[k][1] < v:
                                need[k] = (sh, v)
                        for k, (sh, v) in need.items():
                            eng.wait_ge(sh, v)
                            waited[k] = v
                        ins = fn(eng)
                        ins.then_inc(tok[0], inc)
                    if e == "sp":
                        for (sh, v, _) in final_waits:
                            if waited.get(sh.num, 0) < v:
                                eng.wait_ge(sh, v)
                                waited[sh.num] = v
                getattr(block, engmap[e])(body)


def build_program(n_layers=DEPTH, debug_raw=False):
    nc = bass.Bass("TRN2", target_bir_lowering=False)
    P = Prog(nc)

    def din(name, shape, dt=F32):
        return nc.dram_tensor(name, list(shape), dt, kind="ExternalInput").ap()

    x_in = din("x", [TOK, D])
    cT_in = din("cT", [128, NCH, NB_CORE])
    vecs_in = din("vecs", [128, NV])
    consts_in = din("consts", [128, NCONST])
    wr_in = din("wr", [128, DEPTH, NCH, 20])
    rb_in = din("rb", [1, DEPTH * 20])
    lam_in = din("lamv", [128, 2 * 4 * 64])
    subg_in = din("subgT", [128, 2])
    mod_w = din("mod_w", [DEPTH, D, 6 * D])
    kv_mod_w = din("kv_mod_w", [D, 2 * D])
    conv_in_w = din("conv_in_w", [N_A, D, 3 * D])
    conv_out_w = din("conv_out_w", [N_A, D, D])
    kv_w = din("kv_w", [D, 2 * D])
    q_w = din("q_w", [2, D, D])
    o_w = din("o_w", [2, D, D])
    exp_w1 = din("exp_w1", [DEPTH, NE, D, DE])
    exp_w3 = din("exp_w3", [DEPTH, NE, D, DE])
    exp_w2 = din("exp_w2", [DEPTH, NE, DE, D])
    out = nc.dram_tensor("out", [TOK, D], F32, kind="ExternalOutput").ap()

    xT_d = nc.dram_tensor("xT_d", [128, NCH, TOK], F32).ap()
    kT_d = nc.dram_tensor("kT_d", [128, NH, TOK], BF16).ap()
    v_d = nc.dram_tensor("v_d", [TOK, D], BF16).ap()
    xd_bufs = [Buf(f"xd{i}") for i in range(NST)]
    kd_bufs = [Buf(f"kd{i}") for i in range(NST)]
    vd_bufs = kd_bufs
    out_bufs = [Buf(f"od{i}") for i in range(NST)]

    cur = [16512]

    def salloc(name, shape, dt, at=None):
        nbytes = int(np.prod(shape[1:])) * (4 if dt == F32 else 2)
        if at is None:
            off = cur[0]
            cur[0] += (nbytes + 31) // 32 * 32
        else:
            off = at
        assert off + nbytes <= 229344, (name, off, nbytes)
        return nc.alloc_sbuf_tensor_at(name, list(shape), dt, offset=off)

    consts = salloc("consts", [128, NCONST], F32)
    ident_b = salloc("ident_b", [128, 128], BF16)
    ones_b = salloc("ones_b", [128, 128], BF16)
    ones_f = salloc("ones_f", [128, 128], F32)
    vecs = salloc("vecs", [128, NV], F32)
    wr_s = salloc("wr_s", [128, DEPTH, NCH, 20], F32)
    rb_s = salloc("rb_s", [1, DEPTH * 20], F32)
    cact = salloc("cact", [128, NCH, NB_CORE], F32)
    modT = salloc("modT", [128, 4 * 48 + 16, NB_CORE], F32)
    gm = salloc("gm", [128, 9, NCH, NB_CORE], F32)
    neglam = salloc("neglam", [128, 2], F32)
    subg = salloc("subgT", [128, 2], F32)
    zhist = salloc("zhist", [128, NCH, 2], F32)
    rstd = salloc("rstd", [128, TS], F32)
    xT = salloc("xT", [128, NCH, TS], F32)
    hT = salloc("hT", [128, NCH, TS], BF16)
    ring_base = cur[0]
    cur[0] += 6 * 8192
    eps_t = salloc("eps_t", [128, 1], F32)
    tmp32 = salloc("tmp32", [128, 2, 512], F32)
    bias_sb = salloc("bias_sb", [128, 256], F32)
    U0 = cur[0]
    assert U0 + 76 * 1024 <= 229344, U0

    B_consts, B_identb, B_onesb, B_onesf, B_vecs, B_wr, B_rb = (Buf(n) for n in
                                                                 ("consts", "identb", "onesb", "onesf", "vecs", "wr", "rb"))
    B_cact, B_modT, B_gm, B_neglam, B_subg = (Buf(n) for n in ("cact", "modT", "gm", "neglam", "subg"))
    B_zh = [Buf(f"zh{j}") for j in range(NCH)]
    B_rstd = Buf("rstd")
    B_xT = [[Buf(f"xT{c}_{h}") for h in range(2)] for c in range(NCH)]
    B_hT = [[Buf(f"hT{c}_{h}") for h in range(2)] for c in range(NCH)]
    B_ring = [Buf(f"ring{i}") for i in range(6)]

    def flat(bb):
        return [b for row in bb for b in row]

    psall = nc.alloc_psum_tensor("psall", [128, 8, 512], F32)

    class BankV:
        def __init__(self, i):
            self.i = i

        def __getitem__(self, idx):
            if not isinstance(idx, tuple):
                idx = (idx,)
            return psall[(idx[0], self.i) + tuple(idx[1:])]

    banks = [BankV(i) for i in range(8)]
    B_bank = [Buf(f"bank{i}") for i in range(8)]

    ident_f = consts[:, C_ID:C_ID + 128]
    Dt = consts[:, C_D:C_D + 512]
    Dm = consts[:, C_DM:C_DM + 512]

    def mm(out_ap, lhsT, rhs, start, stop, reads, writes):
        return P.op("pe", lambda e: e.matmul(out_ap, lhsT, rhs, start=start, stop=stop), reads, writes)

    def tr(out_ap, in_ap, ident, reads, writes):
        return P.op("pe", lambda e: e.transpose(out_ap, in_ap, ident), reads, writes)

    def act(out_ap, in_ap, func, reads, writes, bias=None, scale=None, accum_out=None):
        kw = {}
        if bias is not None:
            kw["bias"] = bias
        if scale is not None:
            kw["scale"] = scale
        if accum_out is not None:
            kw["accum_out"] = accum_out
        return P.op("act", lambda e: e.activation(out_ap, in_ap, func, **kw), reads, writes)

    def tt(eng, out_ap, in0, in1, op, reads, writes):
        return P.op(eng, lambda e: e.tensor_tensor(out_ap, in0, in1, op), reads, writes)

    def ts(eng, out_ap, in0, s1, s2, op0, op1, reads, writes):
        if s2 is None:
            return P.op(eng, lambda e: e.tensor_scalar(out_ap, in0, s1, None, op0), reads, writes)
        return P.op(eng, lambda e: e.tensor_scalar(out_ap, in0, s1, s2, op0, op1), reads, writes)

    def stt(out_ap, in0, scalar, in1, op0, op1, reads, writes):
        return P.op("dve", lambda e: e.scalar_tensor_tensor(out_ap, in0, scalar, in1, op0, op1), reads, writes)

    def cp(eng, out_ap, in_ap, reads, writes):
        if eng == "act":
            return P.op("act", lambda e: e.activation(out_ap, in_ap, AF.Copy), reads, writes)
        return P.op(eng, lambda e: e.tensor_copy(out_ap, in_ap), reads, writes)

    def red(out_ap, in_ap, op, reads, writes):
        return P.op("dve", lambda e: e.tensor_reduce(out_ap, in_ap, AX.X, op), reads, writes)

    def recip(out_ap, in_ap, reads, writes):
        return P.op("dve", lambda e: e.reciprocal(out_ap, in_ap), reads, writes)

    def dma(q, out_ap, in_ap, reads, writes, gid=None, exempt=False, noncontig=False):
        if noncontig:
            return P.dma(q, lambda e: e.dma_start(out=out_ap, in_=in_ap, allow_slow_non_contiguous=True), reads, writes, gid, exempt)
        return P.dma(q, lambda e: e.dma_start(out=out_ap, in_=in_ap), reads, writes, gid, exempt)

    BUFS = {}

    def GB(name):
        if name not in BUFS:
            BUFS[name] = Buf(name)
        return BUFS[name]

    gidc = [0]

    def newgid():
        gidc[0] += 1
        return gidc[0]

    dma("sp", consts[:], consts_in[:, :], [], [B_consts])
    dma("sp", vecs[:], vecs_in[:, :], [], [B_vecs])
    dma("sp", wr_s[:], wr_in[:, :, :, :], [], [B_wr])
    dma("sp", rb_s[:], rb_in[:, :], [], [B_rb])
    dma("sp", cact[:], cT_in[:, :, :], [], [B_cact])
    dma("sp", subg[:], subg_in[:, :], [], [B_subg])
    cp("dve", ident_b[:], ident_f, [B_consts], [B_identb])
    P.op("dve", lambda e: e.memset(ones_b[:], 1.0), [], [B_onesb])
    P.op("dve", lambda e: e.memset(ones_f[:], 1.0), [], [B_onesf])
    for j in range(NCH):
        P.op("dve", lambda e, j=j: e.memset(zhist[:, j, :], 0.0), [], [B_zh[j]])
    act(cact[:], cact[:], AF.Silu, [B_cact], [B_cact])

    lam_s = salloc("lam_s", [128, 2, 4, 64], F32, at=U0)
    lam_t = salloc("lam_t", [128, 2, 2, 64], F32, at=U0 + 2048)
    lam_r = salloc("lam_r", [128, 2, 2], F32, at=U0 + 3072)
    B_lam = Buf("lam")
    dma("sp", lam_s[:], lam_in[:, :].rearrange("p (l k d) -> p l k d", l=2, k=4), [], [B_lam])
    for j in range(2):
        tt("dve", lam_t[:, j, 0, :], lam_s[:, j, 0, :], lam_s[:, j, 1, :], ALU.mult, [B_lam], [B_lam])
        tt("dve", lam_t[:, j, 1, :], lam_s[:, j, 2, :], lam_s[:, j, 3, :], ALU.mult, [B_lam], [B_lam])
        red(lam_r[:, j, :], lam_t[:, j, :, :], ALU.add, [B_lam], [B_lam])
        act(lam_r[:, j, :], lam_r[:, j, :], AF.Exp, [B_lam], [B_lam])
        lam_init = 0.8 - 0.6 * float(np.exp(-0.3 * (j + N_A)))
        stt(neglam[:, j:j + 1], lam_r[:, j, 1:2], -lam_init, lam_r[:, j, 0:1], ALU.add, ALU.subtract,
            [B_lam], [B_neglam])

    stage = [salloc(f"stage{i}", [128, NCH, 512], F32, at=U0 + 8192 + i * 16384) for i in range(2)]
    B_stage = [Buf("stage0"), Buf("stage1")]
    pieces = []
    for l in range(DEPTH):
        for pc in range(12):
            pieces.append((mod_w[l], pc, l * 48 + pc * 4, V_MODB + l * 48 + pc * 4))
    for pc in range(4):
        pieces.append((kv_mod_w, pc, 192 + pc * 4, V_KVB + pc * 4))
    for i, (src, pc, col0, vcol0) in enumerate(pieces):
        sb, st_ = B_stage[i % 2], stage[i % 2]
        dma("sp", st_[:], src[:, pc * 512:(pc + 1) * 512].rearrange("(kc p) n -> p kc n", p=128), [], [sb])
        bk = i % 2
        for jj in range(4):
            for kc in range(NCH):
                mm(banks[bk][:, jj * 2:jj * 2 + 2], st_[:, kc, jj * 128:(jj + 1) * 128], cact[:, kc, :],
                   kc == 0, kc == NCH - 1, [sb, B_cact], [B_bank[bk]])
        for jj in range(4):
            ts("dve", modT[:, col0 + jj, :], banks[bk][:, jj * 2:jj * 2 + 2], vecs[:, vcol0 + jj:vcol0 + jj + 1], None,
               ALU.add, None, [B_bank[bk], B_vecs], [B_modT])

    def mcol(l, which):
        return l * 48 + which * 8

    for l in range(DEPTH):
        for (slot, which, vg) in ((l, 1, V_GMIX + l * 8), (4 + l, 4, V_GFFN + l * 8)):
            for b in range(NB_CORE):
                stt(gm[:, slot, :, b], modT[:, mcol(l, which):mcol(l, which) + 8, b], 1.0, vecs[:, vg:vg + 8],
                    ALU.add, ALU.mult, [B_modT, B_vecs], [B_gm])
    for b in range(NB_CORE):
        stt(gm[:, 8, :, b], modT[:, 192 + 8:192 + 16, b], 1.0, vecs[:, V_GKV:V_GKV + 8], ALU.add, ALU.mult,
            [B_modT, B_vecs], [B_gm])
    bias_vals = {}
    B_bias = Buf("bias")
    for hd_ in range(NH):
        for kk in range(0, 32):
            v_ = float(np.float32(-SLOPES[hd_] * 128.0 * kk))
            if v_ not in bias_vals:
                k_ = len(bias_vals)
                assert k_ < 256
                bias_vals[v_] = k_
                P.op("dve", lambda e, k_=k_, v_=v_: e.memset(bias_sb[:, k_:k_ + 1], v_), [], [B_bias])
    P.barrier()

    def norm_stats():
        for c in range(NCH):
            for h in range(2):
                act(hT[:, c, h * 512:(h + 1) * 512], xT[:, c, h * 512:(h + 1) * 512], AF.Square,
                    [B_xT[c][h]], [B_hT[c][h]])
        for h in range(2):
            for c in range(NCH):
                mm(banks[6 + h][:, :], ones_b[:, :], hT[:, c, h * 512:(h + 1) * 512], c == 0, c == NCH - 1,
                   [B_onesb, B_hT[c][h]], [B_bank[6 + h]])
        for h in range(2):
            act(rstd[:, h * 512:(h + 1) * 512], banks[6 + h][:, :], AF.Sqrt, [B_bank[6 + h]], [B_rstd],
                bias=eps_ap, scale=1.0 / D)
        recip(rstd[:, :], rstd[:, :], [B_rstd], [B_rstd])

    B_eps = Buf("eps")
    P.op("dve", lambda e: e.memset(eps_t[:], EPS), [], [B_eps])
    eps_ap = eps_t[:, 0:1]

    def norm_mod(gslot, shcol, b, h32=None, B_h32=None, bar_after_stats=False):
        norm_stats()
        if bar_after_stats:
            P.barrier()
        for c in range(NCH):
            for h in range(2):
                sl = slice(h * 512, (h + 1) * 512)
                if h32 is None:
                    stt(tmp32[:, (c * 2 + h) % 2, :], xT[:, c, sl], gm[:, gslot, c, b:b + 1], rstd[:, sl], ALU.mult,
                        ALU.mult, [B_xT[c][h], B_gm, B_rstd, B_eps], [B_tmp32[(c * 2 + h) % 2]])
                    act(hT[:, c, sl], tmp32[:, (c * 2 + h) % 2, :], AF.Identity,
                        [B_tmp32[(c * 2 + h) % 2], B_modT], [B_hT[c][h]],
                        bias=modT[:, shcol + c, b:b + 1], scale=1.0)
                else:
                    stt(h32[:, c, sl], xT[:, c, sl], gm[:, gslot, c, b:b + 1], rstd[:, sl], ALU.mult, ALU.mult,
                        [B_xT[c][h], B_gm, B_rstd, B_eps], [B_h32[c][h]])
                    act(h32[:, c, sl], h32[:, c, sl], AF.Identity, [B_h32[c][h], B_modT], [B_h32[c][h]],
                        bias=modT[:, shcol + c, b:b + 1], scale=1.0)
                    cp("dve" if (c + h) % 2 else "act", hT[:, c, sl], h32[:, c, sl], [B_h32[c][h]], [B_hT[c][h]])

    B_tmp32 = [Buf("tmp32a"), Buf("tmp32b")]

    def load_x(l, st):
        if l == 0:
            xtok = [salloc(f"xtok{i}_{st}", [128, D], F32, at=U0 + i * 4096) for i in range(2)]
            B_xtok = [GB("xtok0"), GB("xtok1")]
            for t8 in range(8):
                i = t8 % 2
                r0 = st * TS + t8 * 128
                dma("sp", xtok[i][:], x_in[r0:r0 + 128, :], [], [B_xtok[i]])
                for c4 in range(2):
                    bk = (t8 * 2 + c4) % 2
                    for cc in range(4):
                        c = c4 * 4 + cc
                        tr(banks[bk][:, cc * 128:(cc + 1) * 128], xtok[i][:, c * 128:(c + 1) * 128], ident_f,
                           [B_xtok[i], B_consts], [B_bank[bk]])
                    h = t8 // 4
                    cp("act" if c4 else "dve", xT[:, c4 * 4:c4 * 4 + 4, t8 * 128:(t8 + 1) * 128],
                       banks[bk][:, :].rearrange("p (c t) -> p c t", c=4), [B_bank[bk]],
                       [B_xT[c4 * 4 + cc][h] for cc in range(4)])
        else:
            g = newgid()
            for c in range(NCH):
                dma("sp", xT[:, c, :], xT_d[:, c, st * TS:(st + 1) * TS], [xd_bufs[st]], [B_xT[c][0], B_xT[c][1]], gid=g)

    def store_x(st):
        g = newgid()
        for c in range(NCH):
            dma("sp", xT_d[:, c, st * TS:(st + 1) * TS], xT[:, c, :], [B_xT[c][0], B_xT[c][1]], [xd_bufs[st]], gid=g)


    vcnt = [0]

    def rview(i, shape):
        vcnt[0] += 1
        assert int(np.prod(shape[1:])) * 2 + (i * 8192) <= 6 * 8192
        return nc.alloc_sbuf_tensor_at(f"rv{vcnt[0]}", list(shape), BF16, offset=ring_base + i * 8192)

    def uview(name, shape, dt, off):
        vcnt[0] += 1
        nbytes = int(np.prod(shape[1:])) * (4 if dt == F32 else 2)
        assert U0 + off + nbytes <= 229344, (name, off, nbytes, U0)
        return nc.alloc_sbuf_tensor_at(f"{name}_{vcnt[0]}", list(shape), dt, offset=U0 + off)

    def wload(pieces_idx, dst_ap, src_ap):
        return dma("pool", dst_ap, src_ap, [], [B_ring[i] for i in pieces_idx], exempt=True)

    def conv_mixer(l, st, b):
        first = (st % 4 == 0)
        zb = uview("zb", [128, NCH, TS], BF16, 0)
        z = uview("z", [128, TS + 2], F32, 16384)
        zc = uview("zc", [128, TS], F32, 16384 + 4128)
        cs = uview("cs", [128, 2, 512], F32, 16384 + 4128 + 4096)
        B_zb = [[GB(f"zb{c}_{h}") for h in range(2)] for c in range(NCH)]
        B_z, B_zc, B_cs = GB("z"), GB("zc"), [GB("cs0"), GB("cs1")]
        norm_mod(l, mcol(l, 0), b)
        P.barrier()
        wsrc = conv_in_w[l].rearrange("(kc p) (g n) -> p kc g n", p=128, g=3)
        wout = rview(4, [128, NCH, D])
        wload([4, 5], wout[:], conv_out_w[l].rearrange("(kc p) n -> p kc n", p=128))
        for j in range(NCH):
            pi = j % 2
            wj = rview(pi, [128, NCH, 3, 128])
            g_ = newgid()
            for g3 in range(3):
                dma("pool", wj[:, :, g3, :], wsrc[:, :, g3, j * 128:(j + 1) * 128], [], [B_ring[pi]], gid=g_, exempt=True)
            if first:
                P.op("dve", lambda e: e.memset(z[:, 0:2], 0.0), [], [B_z])
            else:
                cp("dve", z[:, 0:2], zhist[:, j, :], [B_zh[j]], [B_z])
            for h in range(2):
                sl = slice(h * 512, (h + 1) * 512)
                bb = 3 * h
                for g in (1, 2):
                    for kc in range(NCH):
                        mm(banks[bb + g][:, :], wj[:, kc, g, :], hT[:, kc, sl], kc == 0, kc == NCH - 1,
                           [B_ring[pi], B_hT[kc][h]], [B_bank[bb + g]])
                cp("act", cs[:, h, :], banks[bb + 1][:, :], [B_bank[bb + 1]], [B_cs[h]])
                tt("dve", z[:, 2 + h * 512:2 + (h + 1) * 512], cs[:, h, :], banks[bb + 2][:, :], ALU.mult,
                   [B_cs[h], B_bank[bb + 2]], [B_z])
            for h in range(2):
                sl = slice(h * 512, (h + 1) * 512)
                bb = 3 * h
                for kc in range(NCH):
                    mm(banks[bb][:, :], wj[:, kc, 0, :], hT[:, kc, sl], kc == 0, kc == NCH - 1,
                       [B_ring[pi], B_hT[kc][h]], [B_bank[bb]])
            cw = V_CW + (l * 8 + j) * 3
            ts("dve", zc[:, :], z[:, 2:2 + TS], vecs[:, cw + 2:cw + 3], vecs[:, V_CB + l * 8 + j:V_CB + l * 8 + j + 1],
               ALU.mult, ALU.add, [B_z, B_vecs], [B_zc])
            stt(zc[:, :], z[:, 1:1 + TS], vecs[:, cw + 1:cw + 2], zc[:, :], ALU.mult, ALU.add, [B_z, B_vecs, B_zc], [B_zc])
            stt(zc[:, :], z[:, 0:TS], vecs[:, cw:cw + 1], zc[:, :], ALU.mult, ALU.add, [B_z, B_vecs, B_zc], [B_zc])
            cp("dve", zhist[:, j, :], z[:, TS:TS + 2], [B_z], [B_zh[j]])
            for h in range(2):
                sl = slice(h * 512, (h + 1) * 512)
                tt("dve", zb[:, j, sl], zc[:, sl], banks[3 * h][:, :], ALU.mult, [B_zc, B_bank[3 * h]], [B_zb[j][h]])
        gcol = mcol(l, 2)
        for oc in range(NCH):
            for h in range(2):
                sl = slice(h * 512, (h + 1) * 512)
                bk = 6 + (oc * 2 + h) % 2
                for kc in range(NCH):
                    mm(banks[bk][:, :], wout[:, kc, oc * 128:(oc + 1) * 128], zb[:, kc, sl], kc == 0, kc == NCH - 1,
                       [B_ring[4], B_ring[5], B_zb[kc][h]], [B_bank[bk]])
                stt(xT[:, oc, sl], banks[bk][:, :], modT[:, gcol + oc, b:b + 1], xT[:, oc, sl], ALU.mult, ALU.add,
                    [B_bank[bk], B_modT, B_xT[oc][h]], [B_xT[oc][h]])

    def moe(l, st, b):
        h32 = uview("h32", [128, NCH, TS], F32, 0)
        acc = uview("acc", [128, NCH, TS], F32, 32768)
        hid = uview("hid", [128, 2, 4, 512], BF16, 65536)
        sa = uview("sa", [128, 2, 512], F32, 65536 + 8192)
        o = 65536 + 8192 + 4096
        Ls = uview("Ls", [128, 8, 20], F32, o); o += 640
        r4 = [uview(f"r4{i}", [128, 8, 4], F32, o + i * 128) for i in range(8)]; o += 8 * 128
        r1 = [uview(f"r1{i}", [128, 8], F32, o + i * 32) for i in range(8)]; o += 8 * 32
        t16 = uview("t16", [128, 8, 4, 4], F32, o); o += 512
        comb = uview("comb", [128, 8, 4, 4], F32, o); o += 512
        B_h32 = [[GB(f"h32_{c}_{h}") for h in range(2)] for c in range(NCH)]
        B_acc = [[GB(f"acc_{c}_{h}") for h in range(2)] for c in range(NCH)]
        B_hid = [[GB(f"hid_{h}_{m}") for m in range(4)] for h in range(2)]
        B_sa = [GB("sa0"), GB("sa1")]
        B_r = GB("router")
        norm_mod(4 + l, mcol(l, 3), b, h32, B_h32, bar_after_stats=True)
        LB = banks[7][:, 0:256].rearrange("p (t n) -> p t n", t=8)
        for t8 in range(8):
            h = t8 // 4
            for kc in range(NCH):
                mm(LB[:, t8, 0:20], h32[:, kc, t8 * 128:(t8 + 1) * 128], wr_s[:, l, kc, :], kc == 0, False,
                   [B_h32[kc][h], B_wr], [B_bank[7]])
            mm(LB[:, t8, 0:20], ones_f[0:1, :], rb_s[0:1, l * 20:(l + 1) * 20], False, True, [B_onesf, B_rb], [B_bank[7]])
        cp("dve", Ls[:, :, :], LB[:, :, 0:20], [B_bank[7]], [B_r])
        R = [B_r]
        gl = Ls[:, :, 0:4]
        gmax, gs, pg, m1, m2, w1, w2 = r1[0], r1[1], r1[2], r1[3], r1[4], r1[5], r1[6]
        gsh, oh, esel, d1, mk1, e2, mk2, wexp = r4

        def bc4(a):
            return a[:, :].unsqueeze(2).to_broadcast([128, 8, 4])

        red(gmax[:, :], gl, ALU.max, R, R)
        tt("dve", gsh[:, :, :], gl, bc4(gmax), ALU.subtract, R, R)
        ts("dve", oh[:, :, :], gsh[:, :, :], 0.0, None, ALU.is_equal, None, R, R)
        act(gsh[:, :, :], gsh[:, :, :], AF.Exp, R, R)
        red(gs[:, :], gsh[:, :, :], ALU.add, R, R)
        recip(pg[:, :], gs[:, :], R, R)
        el = Ls[:, :, 4:20].rearrange("p t (g e) -> p t g e", g=4)
        tt("dve", t16[:, :, :, :], el, oh[:, :, :].unsqueeze(3).to_broadcast([128, 8, 4, 4]), ALU.mult, R, R)
        tt("dve", esel[:, :, :], t16[:, :, 0, :], t16[:, :, 1, :], ALU.add, R, R)
        tt("dve", esel[:, :, :], esel[:, :, :], t16[:, :, 2, :], ALU.add, R, R)
        tt("dve", esel[:, :, :], esel[:, :, :], t16[:, :, 3, :], ALU.add, R, R)
        red(m1[:, :], esel[:, :, :], ALU.max, R, R)
        tt("dve", d1[:, :, :], esel[:, :, :], bc4(m1), ALU.subtract, R, R)
        ts("dve", mk1[:, :, :], d1[:, :, :], 0.0, None, ALU.is_equal, None, R, R)
        stt(e2[:, :, :], mk1[:, :, :], -1e30, d1[:, :, :], ALU.mult, ALU.add, R, R)
        red(m2[:, :], e2[:, :, :], ALU.max, R, R)
        tt("dve", d1[:, :, :], e2[:, :, :], bc4(m2), ALU.subtract, R, R)
        ts("dve", mk2[:, :, :], d1[:, :, :], 0.0, None, ALU.is_equal, None, R, R)
        act(w2[:, :], m2[:, :], AF.Exp, R, R)
        ts("dve", w1[:, :], w2[:, :], 1.0, None, ALU.add, None, R, R)
        recip(w1[:, :], w1[:, :], R, R)
        tt("dve", w2[:, :], w2[:, :], w1[:, :], ALU.mult, R, R)
        tt("dve", w1[:, :], w1[:, :], pg[:, :], ALU.mult, R, R)
        tt("dve", w2[:, :], w2[:, :], pg[:, :], ALU.mult, R, R)
        tt("dve", wexp[:, :, :], mk1[:, :, :], bc4(w1), ALU.mult, R, R)
        tt("dve", mk2[:, :, :], mk2[:, :, :], bc4(w2), ALU.mult, R, R)
        tt("dve", wexp[:, :, :], wexp[:, :, :], mk2[:, :, :], ALU.add, R, R)
        tt("dve", comb[:, :, :, :], oh[:, :, :].unsqueeze(3).to_broadcast([128, 8, 4, 4]),
           wexp[:, :, :].unsqueeze(2).to_broadcast([128, 8, 4, 4]), ALU.mult, R, R)
        combf = comb[:, :, :, :].rearrange("p t g e -> p t (g e)")

        wviews = {}

        def S1(u, e, h):
            s3 = 3 * (e % 2)
            if h == 0:
                w1v = rview(s3, [128, NCH, DE])
                w3v = rview(s3 + 1, [128, NCH, DE])
                w2v = rview(s3 + 2, [128, 4, D])
                wload([s3], w1v[:], exp_w1[l, e].rearrange("(kc p) n -> p kc n", p=128))
                wload([s3 + 1], w3v[:], exp_w3[l, e].rearrange("(kc p) n -> p kc n", p=128))
                wload([s3 + 2], w2v[:], exp_w2[l, e].rearrange("(kc p) n -> p kc n", p=128))
                wviews[e] = (w1v, w3v, w2v)
            w1v, w3v, w2v = wviews[e]
            sl = slice(h * 512, (h + 1) * 512)
            bbk = 4 if u % 2 == 0 else 7
            for t4 in range(4):
                t8 = h * 4 + t4
                mm(banks[bbk][:, t4 * 128:(t4 + 1) * 128], combf[:, t8, e:e + 1].to_broadcast([128, 128]), ident_f,
                   True, True, [B_r, B_consts], [B_bank[bbk]])
            for m in range(4):
                ba, bb_ = 2 * (m % 2), 2 * (m % 2) + 1
                for kc in range(NCH):
                    mm(banks[ba][:, :], w1v[:, kc, m * 128:(m + 1) * 128], hT[:, kc, sl], kc == 0, kc == NCH - 1,
                       [B_ring[s3], B_hT[kc][h]], [B_bank[ba]])
                for kc in range(NCH):
                    mm(banks[bb_][:, :], w3v[:, kc, m * 128:(m + 1) * 128], hT[:, kc, sl], kc == 0, kc == NCH - 1,
                       [B_ring[s3 + 1], B_hT[kc][h]], [B_bank[bb_]])
                act(sa[:, m % 2, :], banks[ba][:, :], AF.Silu, [B_bank[ba]], [B_sa[m % 2]])
                tt("dve", sa[:, m % 2, :], sa[:, m % 2, :], banks[bb_][:, :], ALU.mult, [B_sa[m % 2], B_bank[bb_]],
                   [B_sa[m % 2]])
                tt("dve", hid[:, h, m, :], sa[:, m % 2, :], banks[bbk][:, :], ALU.mult, [B_sa[m % 2], B_bank[bbk]],
                   [B_hid[h][m]])

        def S2(u, e, h):
            s3 = 3 * (e % 2)
            w1v, w3v, w2v = wviews[e]
            sl = slice(h * 512, (h + 1) * 512)
            for j in range(NCH):
                bo = 5 + (j % 2)
                for m in range(4):
                    mm(banks[bo][:, :], w2v[:, m, j * 128:(j + 1) * 128], hid[:, h, m, :], m == 0, m == 3,
                       [B_ring[s3 + 2], B_hid[h][m]], [B_bank[bo]])
                if e == 0:
                    cp("act", acc[:, j, sl], banks[bo][:, :], [B_bank[bo]], [B_acc[j][h]])
                else:
                    tt("dve", acc[:, j, sl], acc[:, j, sl], banks[bo][:, :], ALU.add, [B_acc[j][h], B_bank[bo]],
                       [B_acc[j][h]])

        units = [(e, h) for e in range(NE) for h in range(2)]
        for u, (e, h) in enumerate(units):
            S1(u, e, h)
            if u >= 1:
                S2(u - 1, *units[u - 1])
        S2(len(units) - 1, *units[-1])
        gcol = mcol(l, 5)
        for j in range(NCH):
            for h in range(2):
                sl = slice(h * 512, (h + 1) * 512)
                stt(xT[:, j, sl], acc[:, j, sl], modT[:, gcol + j, b:b + 1], xT[:, j, sl], ALU.mult, ALU.add,
                    [B_acc[j][h], B_modT, B_xT[j][h]], [B_xT[j][h]])

    def kv_stage(st, b):
        kst = uview("kst", [128, NH, TS], BF16, 0)
        vst = uview("vst", [128, 8, D], BF16, 16384)
        B_kst = [GB(f"kst{i}") for i in range(NH)]
        B_vst = [GB(f"vst{i}") for i in range(8)]
        norm_mod(8, 192, b)
        wk = rview(0, [128, NCH, D])
        wv = rview(2, [128, NCH, D])
        wload([0, 1], wk[:], kv_w[:, 0:D].rearrange("(kc p) n -> p kc n", p=128))
        wload([2, 3], wv[:], kv_w[:, D:2 * D].rearrange("(kc p) n -> p kc n", p=128))
        n = 0
        for hd in range(NH):
            for h in range(2):
                sl = slice(h * 512, (h + 1) * 512)
                bk = n % 4; n += 1
                for kc in range(NCH):
                    mm(banks[bk][:, :], wk[:, kc, hd * 128:(hd + 1) * 128], hT[:, kc, sl], kc == 0, kc == NCH - 1,
                       [B_ring[0], B_ring[1], B_hT[kc][h]], [B_bank[bk]])
                cp("act" if n % 2 else "dve", kst[:, hd, sl], banks[bk][:, :], [B_bank[bk]], [B_kst[hd]])
        g = newgid()
        for hd in range(NH):
            dma("sp", kT_d[:, hd, st * TS:(st + 1) * TS], kst[:, hd, :], [B_kst[hd]], [kd_bufs[st]], gid=g)
        for t8 in range(8):
            h = t8 // 4
            for nh in range(2):
                bk = n % 4; n += 1
                for kc in range(NCH):
                    mm(banks[bk][:, :], hT[:, kc, t8 * 128:(t8 + 1) * 128], wv[:, kc, nh * 512:(nh + 1) * 512], kc == 0,
                       kc == NCH - 1, [B_ring[2], B_ring[3], B_hT[kc][h]], [B_bank[bk]])
                cp("act" if n % 2 else "dve", vst[:, t8, nh * 512:(nh + 1) * 512], banks[bk][:, :], [B_bank[bk]],
                   [B_vst[t8]])
        for t8 in range(8):
            r0 = st * TS + t8 * 128
            dma("sp", v_d[r0:r0 + 128, :], vst[:, t8, :], [B_vst[t8]], [vd_bufs[st]], gid=g)

    def attn(l, st, b):
        ja = l - N_A
        lam_init = 0.8 - 0.6 * float(np.exp(-0.3 * l))
        sq = st % 4
        s0 = (st // 4) * 4
        nkeys = (sq + 1) * TS
        nkt_all = nkeys // 128
        NPB, NTB, LOOK = 4, 3, 2
        qT = uview("qT", [128, NH, TS], BF16, 0)
        kh = [uview(f"kh{i}", [128, SEQ], BF16, 16384 + i * 8192) for i in range(2)]
        vh = [uview(f"vh{i}", [128, 32, 128], BF16, 32768 + i * 8192) for i in range(2)]
        pb = [uview(f"pb{i}", [128, 2, 512], BF16, 49152 + i * 2048) for i in range(NPB)]
        o = 49152 + NPB * 2048
        tb = [uview(f"tb{i}", [128, 2, 512], F32, o + i * 4096) for i in range(NTB)]
        o += NTB * 4096
        r1 = uview("r1", [128, 512], F32, o); o += 2048
        r2 = uview("r2", [128, 512], F32, o); o += 2048
        e1 = uview("e1", [128, 512], F32, o); o += 2048
        e2 = uview("e2", [128, 512], F32, o); o += 2048
        sqb = uview("sqb", [128, 512], BF16, o); o += 1024
        subs = uview("subs", [128, 1], F32, o); o += 32
        B_q = [[GB(f"q{a}_{h}") for h in range(2)] for a in range(NH)]
        B_kh, B_vh = [GB("kh0"), GB("kh1")], [GB("vh0"), GB("vh1")]
        B_pb, B_tb = [GB(f"pb{i}") for i in range(NPB)], [GB(f"tb{i}") for i in range(NTB)]
        B_r1, B_r2, B_e1, B_e2, B_sqb, B_subs = (GB(n) for n in ("ar1", "ar2", "ae1", "ae2", "asqb", "asubs"))
        norm_mod(l, mcol(l, 0), b)
        P.barrier()
        wq = rview(0, [128, NCH, D])
        wload([0, 1], wq[:], q_w[ja].rearrange("(kc p) n -> p kc n", p=128))
        wo = rview(2, [128, NCH, D])
        wload([2, 3], wo[:], o_w[ja].rearrange("(kc p) n -> p kc n", p=128))
        ts("dve", subs[:, :], subg[:, ja:ja + 1], 1.0 - lam_init, None, ALU.mult, None, [B_subg], [B_subs])

        def load_kv(hd):
            ki = hd % 2
            tk0 = s0 * TS
            dma("sp", kh[ki][:, 0:nkeys], kT_d[:, hd, tk0:tk0 + nkeys], kd_bufs[s0:s0 + sq + 1], [B_kh[ki]])
            dma("sp", vh[ki][:, 0:nkt_all, :],
                v_d[tk0:tk0 + nkeys, hd * 128:(hd + 1) * 128].rearrange("(kt p) d -> p kt d", p=128),
                vd_bufs[s0:s0 + sq + 1], [B_vh[ki]])

        load_kv(0)
        load_kv(1)
        n = 0
        for hd in range(NH):
            for h in range(2):
                sl = slice(h * 512, (h + 1) * 512)
                bk = n % 2; n += 1
                for kc in range(NCH):
                    mm(banks[bk][:, :], wq[:, kc, hd * 128:(hd + 1) * 128], hT[:, kc, sl], kc == 0, kc == NCH - 1,
                       [B_ring[0], B_ring[1], B_hT[kc][h]], [B_bank[bk]])
                cp("act" if n % 2 else "dve", qT[:, hd, sl], banks[bk][:, :], [B_bank[bk]], [B_q[hd][h]])
        tiles = []
        for hd in range(NH):
            for h in range(2):
                q0 = sq * TS + h * 512
                nkt = q0 // 128 + 4
                for kt in range(nkt):
                    tiles.append((hd, h, kt, q0, nkt))

        def front(n, t):
            hd, h, kt, q0, nkt = t
            ki = hd % 2
            slope = SLOPES[hd]
            jd = kt - q0 // 128
            qoff = 128 * jd if jd > 0 else 0
            nn = 512 - qoff
            sp, pi, ti = 2 * (n % 2), n % NPB, n % NTB
            for s in range(2):
                ps = slice(s * 64, (s + 1) * 64)
                mm(banks[sp + s][:, 0:nn], kh[ki][ps, kt * 128:(kt + 1) * 128],
                   qT[ps, hd, h * 512 + qoff:(h + 1) * 512], True, True, [B_kh[ki], B_q[hd][h]], [B_bank[sp + s]])
            dsel = Dm if jd >= 0 else Dt
            stt(tb[ti][:, :, 0:nn], dsel[:, 0:nn].unsqueeze(1).to_broadcast([128, 2, nn]), -slope * 8.0,
                psall[:, sp:sp + 2, 0:nn], ALU.mult, ALU.add, [B_consts, B_bank[sp], B_bank[sp + 1]], [B_tb[ti]])
            cb = -slope * float(q0 - kt * 128) if jd < 0 else 0.0
            act(pb[pi][:, :, qoff:512], tb[ti][:, :, 0:nn], AF.Exp, [B_tb[ti], B_bias], [B_pb[pi]],
                bias=bias_tile(cb), scale=0.125)

        def back(n, t):
            hd, h, kt, q0, nkt = t
            ki = hd % 2
            jd = kt - q0 // 128
            qoff = 128 * jd if jd > 0 else 0
            pi = n % NPB
            first, last = (kt == 0), (kt == nkt - 1)
            for s in range(2):
                mm(banks[4 + s][:, qoff:512], vh[ki][:, kt, :], pb[pi][:, s, qoff:512], first, last,
                   [B_vh[ki], B_pb[pi]], [B_bank[4 + s]])
            for s in range(2):
                mm(banks[6 + s][:, qoff:512], ones_b[:, :], pb[pi][:, s, qoff:512], first, last,
                   [B_onesb, B_pb[pi]], [B_bank[6 + s]])
            if not last:
                return
            recip(r1[:, :], banks[6][:, :], [B_bank[6]], [B_r1])
            recip(r2[:, :], banks[7][:, :], [B_bank[7]], [B_r2])
            tt("dve", e1[:, :], banks[4][:, :], r1[:, :], ALU.mult, [B_bank[4], B_r1], [B_e1])
            stt(e2[:, :], banks[5][:, :], neglam[:, ja:ja + 1], r2[:, :], ALU.mult, ALU.mult,
                [B_bank[5], B_neglam, B_r2], [B_e2])
            tt("pool", e1[:, :], e1[:, :], e2[:, :], ALU.add, [B_e1, B_e2], [B_e1])
            act(sqb[:, :], e1[:, :], AF.Square, [B_e1], [B_sqb])
            pending.append((n + LOOK + DELAY, hd, h))
            if h == 1 and hd + 2 < NH:
                load_kv(hd + 2)

        def evac2(hd, h, bk):
            sl = slice(h * 512, (h + 1) * 512)
            mm(banks[bk][:, :], ones_b[:, :], sqb[:, :], True, True, [B_onesb, B_sqb], [B_bank[bk]])
            act(r1[:, :], banks[bk][:, :], AF.Sqrt, [B_bank[bk], B_eps], [B_r1], bias=eps_ap, scale=1.0 / 128)
            recip(r1[:, :], r1[:, :], [B_r1], [B_r1])
            stt(hT[:, hd, sl], e1[:, :], subs[:, 0:1], r1[:, :], ALU.mult, ALU.mult, [B_e1, B_subs, B_r1],
                [B_hT[hd][h]])

        DELAY = 2
        pending = []
        for n, t in enumerate(tiles):
            front(n, t)
            if n >= LOOK:
                back(n - LOOK, tiles[n - LOOK])
            while pending and pending[0][0] <= n:
                _, hd_, h_ = pending.pop(0)
                evac2(hd_, h_, 2 * ((n + 1) % 2))
        for n in range(max(len(tiles) - LOOK, 0), len(tiles)):
            back(n, tiles[n])
        for (_, hd_, h_) in pending:
            evac2(hd_, h_, 0)
        gcol = mcol(l, 2)
        for oc in range(NCH):
            for h in range(2):
                sl = slice(h * 512, (h + 1) * 512)
                bk = (oc * 2 + h) % 2
                for kc in range(NCH):
                    mm(banks[bk][:, :], wo[:, kc, oc * 128:(oc + 1) * 128], hT[:, kc, sl], kc == 0, kc == NCH - 1,
                       [B_ring[2], B_ring[3], B_hT[kc][h]], [B_bank[bk]])
                stt(xT[:, oc, sl], banks[bk][:, :], modT[:, gcol + oc, b:b + 1], xT[:, oc, sl], ALU.mult, ALU.add,
                    [B_bank[bk], B_modT, B_xT[oc][h]], [B_xT[oc][h]])

    def bias_tile(v):
        k = bias_vals[float(np.float32(v))]
        return bias_sb[:, k:k + 1]

    def final_out(st, raw):
        y32 = uview("y32", [128, NCH, TS], F32, 0)
        otok = [uview(f"otok{i}", [128, D], F32, 32768 + i * 4096) for i in range(2)]
        B_y = [[GB(f"y{c}_{h}") for h in range(2)] for c in range(NCH)]
        B_ot = [GB("ot0"), GB("ot1")]
        if not raw:
            norm_stats()
        for c in range(NCH):
            for h in range(2):
                sl = slice(h * 512, (h + 1) * 512)
                if raw:
                    cp("dve", y32[:, c, sl], xT[:, c, sl], [B_xT[c][h]], [B_y[c][h]])
                else:
                    stt(y32[:, c, sl], xT[:, c, sl], vecs[:, V_GFIN + c:V_GFIN + c + 1], rstd[:, sl], ALU.mult, ALU.mult,
                        [B_xT[c][h], B_vecs, B_rstd], [B_y[c][h]])
        g = newgid()
        n = 0
        for t8 in range(8):
            h = t8 // 4
            i = t8 % 2
            for c4 in range(2):
                bk = n % 2; n += 1
                for cc in range(4):
                    c = c4 * 4 + cc
                    tr(banks[bk][:, cc * 128:(cc + 1) * 128], y32[:, c, t8 * 128:(t8 + 1) * 128], ident_f,
                       [B_y[c][h], B_consts], [B_bank[bk]])
                cp("act" if c4 else "dve", otok[i][:, c4 * 512:(c4 + 1) * 512], banks[bk][:, :], [B_bank[bk]], [B_ot[i]])
            r0 = st * TS + t8 * 128
            dma("sp", out[r0:r0 + 128, :], otok[i][:], [B_ot[i]], [out_bufs[st]], gid=g)

    for l in range(n_layers):
        for st in range(NST):
            b = st // 4
            if l == 0:
                P.barrier()
            load_x(l, st)
            if l < N_A:
                conv_mixer(l, st, b)
            else:
                attn(l, st, b)
            moe(l, st, b)
            if l == N_A - 1 and n_layers > N_A:
                P.barrier()
                kv_stage(st, b)
            if l == n_layers - 1:
                P.barrier()
                final_out(st, raw=(debug_raw or n_layers < DEPTH))
            else:
                store_x(st)
    finals = [bf.w for bf in out_bufs if bf.w is not None]
    P.emit(finals)
    return nc, P


def _fm(v):
    v = np.asarray(v, np.float32).reshape(-1, 128)
    return np.ascontiguousarray(v.T)


def _host_layout(inputs, core):
    f32 = np.float32
    b0 = core * NB_CORE
    m = {}
    m["x"] = np.ascontiguousarray(inputs["x"][b0:b0 + NB_CORE].reshape(TOK, D))
    c2 = np.asarray(inputs["c"][b0:b0 + NB_CORE], f32)
    m["cT"] = np.ascontiguousarray(c2.T.reshape(NCH, 128, NB_CORE).transpose(1, 0, 2))
    vec = np.zeros((128, NV), f32)
    vec[:, V_MODB:V_MODB + 192] = _fm(inputs["mod_b"])
    vec[:, V_KVB:V_KVB + 16] = _fm(inputs["kv_mod_b"])
    vec[:, V_GMIX:V_GMIX + 32] = _fm(inputs["norm_mix_g"])
    vec[:, V_GFFN:V_GFFN + 32] = _fm(inputs["norm_ffn_g"])
    vec[:, V_GKV:V_GKV + 8] = _fm(inputs["kv_norm_g"])
    vec[:, V_GFIN:V_GFIN + 8] = _fm(inputs["final_norm_g"])
    cw = np.asarray(inputs["conv_w"], f32)
    vec[:, V_CW:V_CW + 48] = cw.reshape(2, 3, NCH, 128).transpose(3, 0, 2, 1).reshape(128, 48)
    vec[:, V_CB:V_CB + 16] = _fm(inputs["conv_b"])
    m["vecs"] = vec
    k = np.arange(128)[:, None].astype(f32)
    q = np.arange(512)[None, :].astype(f32)
    cst = np.zeros((128, NCONST), f32)
    cst[:, C_ID:C_ID + 128] = np.eye(128, dtype=f32)
    cst[:, C_D:C_D + 512] = q - k
    dmk = (q - k).copy()
    dmk[(q - k) < 0] = 1e30
    cst[:, C_DM:C_DM + 512] = dmk
    m["consts"] = cst
    wr = np.concatenate([np.asarray(inputs["router_group_w"], f32), np.asarray(inputs["router_exp_w"], f32)], axis=-1)
    m["wr"] = np.ascontiguousarray(wr.reshape(DEPTH, NCH, 128, 20).transpose(2, 0, 1, 3))
    rb = np.concatenate([np.asarray(inputs["router_group_b"], f32), np.asarray(inputs["router_exp_b"], f32)], axis=-1)
    m["rb"] = np.ascontiguousarray(rb.reshape(1, DEPTH * 20))
    lam = np.stack([inputs["lam_q1"], inputs["lam_k1"], inputs["lam_q2"], inputs["lam_k2"]], axis=1)
    m["lamv"] = np.ascontiguousarray(np.broadcast_to(np.asarray(lam, f32).reshape(1, -1), (128, 512)))
    m["subgT"] = np.ascontiguousarray(np.asarray(inputs["subln_g"], f32).T)
    for kname in ("mod_w", "kv_mod_w", "conv_in_w", "conv_out_w", "kv_w", "q_w", "o_w", "exp_w1", "exp_w3", "exp_w2"):
        m[kname] = np.ascontiguousarray(np.asarray(inputs[kname], f32))
    return m


def kernel(**inputs):
    nc, _ = build_program()
    in_maps = [_host_layout(inputs, core) for core in range(8)]
    res = run_bass_kernel_spmd(nc, in_maps, core_ids=list(range(8)))
    outs = [np.asarray(r["out"], np.float32).reshape(NB_CORE, SEQ, D) for r in res.results]
    return np.concatenate(outs, axis=0)
```

```python
import numpy as np
import concourse.bass as bass
import concourse.mybir as mybir
from concourse.bass_utils import run_bass_kernel_spmd

F32 = mybir.dt.float32
BF16 = mybir.dt.bfloat16
AF = mybir.ActivationFunctionType
ALU = mybir.AluOpType
AX = mybir.AxisListType

D = 1024
NCH = 8
SEQ = 4096
NB_CORE = 2
TOK = NB_CORE * SEQ
TS = 1024
NST = TOK // TS
DEPTH = 4
N_A = 2
NH = 8
NE = 16
DE = 512
EPS = 1e-6
SLOPES = [2.0 ** (-8.0 * (h + 1) / NH) for h in range(NH)]

V_MODB = 0
V_KVB = V_MODB + 4 * 48
V_GMIX = V_KVB + 16
V_GFFN = V_GMIX + 32
V_GKV = V_GFFN + 32
V_GFIN = V_GKV + 8
V_CW = V_GFIN + 8
V_CB = V_CW + 48
NV = V_CB + 16

C_ID = 0
C_D = 128
C_DM = 128 + 512
NCONST = 128 + 1024


class Buf:
    __slots__ = ("name", "w", "r", "sem", "cnt")

    def __init__(self, name):
        self.name = name
        self.w = None
        self.r = {}
        self.sem = None
        self.cnt = 0


class Prog:
    ENGS = ("pe", "act", "dve", "pool", "sp")

    def __init__(self, nc):
        self.nc = nc
        self.ops = {e: [] for e in self.ENGS}
        self.semh = {}
        self.cnt = {}
        self.nsem = 0
        for e in ("pe", "act", "dve", "pool"):
            self._newsem(e)
        self.last = {}
        self.pend_bar = {e: [] for e in self.ENGS}
        self.dma_pending = []
        self.nops = 0

    def _alloc_sem(self, name):
        self.nsem += 1
        return self.nc.alloc_semaphore(f"s{self.nsem}_{name}")

    def _newsem(self, e):
        self.semh[e] = self._alloc_sem(e)
        self.cnt[e] = 0

    def _tick(self, e):
        if self.cnt[e] >= 30000:
            self._newsem(e)
        self.cnt[e] += 1
        tok = (self.semh[e], self.cnt[e], e)
        self.last[e] = tok
        return tok

    def _deps(self, reads, writes, gid=None):
        waits = []
        for b in reads:
            if b.w is not None:
                waits.append(b.w)
        for b in writes:
            if b.w is not None and not (gid is not None and b.w[2] == ("dma", gid)):
                waits.append(b.w)
            waits.extend(b.r.values())
        return waits

    def _record(self, tok, reads, writes):
        key = (tok[0].num, tok[2])
        for b in reads:
            b.r[key] = tok
        for b in writes:
            b.w = tok
            b.r = {}

    def op(self, eng, fn, reads=(), writes=()):
        waits = self._deps(reads, writes)
        if self.pend_bar[eng]:
            waits.extend(self.pend_bar[eng])
            self.pend_bar[eng] = []
        tok = self._tick(eng)
        self.ops[eng].append((waits, fn, tok, 1))
        self._record(tok, reads, writes)
        self.nops += 1
        return tok

    def dma(self, q, fn, reads=(), writes=(), gid=None, exempt=False):
        b0 = writes[0]
        waits = self._deps(reads, writes, gid)
        if self.pend_bar[q] and not exempt:
            waits.extend(self.pend_bar[q])
            self.pend_bar[q] = []
        if b0.sem is None or b0.cnt >= 30000:
            b0.sem = self._alloc_sem("d" + b0.name)
            b0.cnt = 0
        b0.cnt += 16
        tok = (b0.sem, b0.cnt, ("dma", gid))
        self.ops[q].append((waits, fn, tok, 16))
        self._record(tok, reads, writes)
        if not exempt:
            self.dma_pending.append(tok)
        self.nops += 1
        return tok

    def barrier(self):
        toks = [self.last[e] for e in ("pe", "act", "dve", "pool") if e in self.last]
        toks.extend(self.dma_pending)
        self.dma_pending = []
        for e in self.ENGS:
            self.pend_bar[e] = list(self.pend_bar[e]) + toks

    def emit(self, final_waits):
        nc = self.nc
        engmap = {"pe": "tensor", "act": "scalar", "dve": "vector", "pool": "gpsimd", "sp": "sync"}
        with nc.Block() as block:
            for e in self.ENGS:
                def body(eng, e=e):
                    waited = {}
                    for waits, fn, tok, inc in self.ops[e]:
                        need = {}
                        for (sh, v, pe) in waits:
                            if pe == "pe" and e == "pe":
                                continue
                            k = sh.num
                            if waited.get(k, 0) >= v:
                                continue
                            if k not in need or need[k][1] < v:
                                need[k] = (sh, v)
                        for k, (sh, v) in need.items():
                            eng.wait_ge(sh, v)
                            waited[k] = v
                        ins = fn(eng)
                        ins.then_inc(tok[0], inc)
                    if e == "sp":
                        for (sh, v, _) in final_waits:
                            if waited.get(sh.num, 0) < v:
                                eng.wait_ge(sh, v)
                                waited[sh.num] = v
                getattr(block, engmap[e])(body)


def build_program(n_layers=DEPTH, debug_raw=False):
    nc = bass.Bass("TRN2", target_bir_lowering=False)
    P = Prog(nc)

    def din(name, shape, dt=F32):
        return nc.dram_tensor(name, list(shape), dt, kind="ExternalInput").ap()

    x_in = din("x", [TOK, D])
    cT_in = din("cT", [128, NCH, NB_CORE])
    vecs_in = din("vecs", [128, NV])
    consts_in = din("consts", [128, NCONST])
    wr_in = din("wr", [128, DEPTH, NCH, 20])
    rb_in = din("rb", [1, DEPTH * 20])
    lam_in = din("lamv", [128, 2 * 4 * 64])
    subg_in = din("subgT", [128, 2])
    mod_w = din("mod_w", [DEPTH, D, 6 * D])
    kv_mod_w = din("kv_mod_w", [D, 2 * D])
    conv_in_w = din("conv_in_w", [N_A, D, 3 * D])
    conv_out_w = din("conv_out_w", [N_A, D, D])
    kv_w = din("kv_w", [D, 2 * D])
    q_w = din("q_w", [2, D, D])
    o_w = din("o_w", [2, D, D])
    exp_w1 = din("exp_w1", [DEPTH, NE, D, DE])
    exp_w3 = din("exp_w3", [DEPTH, NE, D, DE])
    exp_w2 = din("exp_w2", [DEPTH, NE, DE, D])
    out = nc.dram_tensor("out", [TOK, D], F32, kind="ExternalOutput").ap()

    xT_d = nc.dram_tensor("xT_d", [128, NCH, TOK], F32).ap()
    kT_d = nc.dram_tensor("kT_d", [128, NH, TOK], BF16).ap()
    v_d = nc.dram_tensor("v_d", [TOK, D], BF16).ap()
    xd_bufs = [Buf(f"xd{i}") for i in range(NST)]
    kd_bufs = [Buf(f"kd{i}") for i in range(NST)]
    vd_bufs = kd_bufs
    out_bufs = [Buf(f"od{i}") for i in range(NST)]

    cur = [16512]

    def salloc(name, shape, dt, at=None):
        nbytes = int(np.prod(shape[1:])) * (4 if dt == F32 else 2)
        if at is None:
            off = cur[0]
            cur[0] += (nbytes + 31) // 32 * 32
        else:
            off = at
        assert off + nbytes <= 229344, (name, off, nbytes)
        return nc.alloc_sbuf_tensor_at(name, list(shape), dt, offset=off)

    consts = salloc("consts", [128, NCONST], F32)
    ident_b = salloc("ident_b", [128, 128], BF16)
    ones_b = salloc("ones_b", [128, 128], BF16)
    ones_f = salloc("ones_f", [128, 128], F32)
    vecs = salloc("vecs", [128, NV], F32)
    wr_s = salloc("wr_s", [128, DEPTH, NCH, 20], F32)
    rb_s = salloc("rb_s", [1, DEPTH * 20], F32)
    cact = salloc("cact", [128, NCH, NB_CORE], F32)
    modT = salloc("modT", [128, 4 * 48 + 16, NB_CORE], F32)
    gm = salloc("gm", [128, 9, NCH, NB_CORE], F32)
    neglam = salloc("neglam", [128, 2], F32)
    subg = salloc("subgT", [128, 2], F32)
    zhist = salloc("zhist", [128, NCH, 2], F32)
    rstd = salloc("rstd", [128, TS], F32)
    xT = salloc("xT", [128, NCH, TS], F32)
    hT = salloc("hT", [128, NCH, TS], BF16)
    ring_base = cur[0]
    cur[0] += 6 * 8192
    eps_t = salloc("eps_t", [128, 1], F32)
    tmp32 = salloc("tmp32", [128, 2, 512], F32)
    bias_sb = salloc("bias_sb", [128, 256], F32)
    U0 = cur[0]
    assert U0 + 76 * 1024 <= 229344, U0

    B_consts, B_identb, B_onesb, B_onesf, B_vecs, B_wr, B_rb = (Buf(n) for n in
                                                                 ("consts", "identb", "onesb", "onesf", "vecs", "wr", "rb"))
    B_cact, B_modT, B_gm, B_neglam, B_subg = (Buf(n) for n in ("cact", "modT", "gm", "neglam", "subg"))
    B_zh = [Buf(f"zh{j}") for j in range(NCH)]
    B_rstd = [Buf("rstd0"), Buf("rstd1")]
    B_xT = [[Buf(f"xT{c}_{h}") for h in range(2)] for c in range(NCH)]
    B_hT = [[Buf(f"hT{c}_{h}") for h in range(2)] for c in range(NCH)]
    B_ring = [Buf(f"ring{i}") for i in range(6)]

    def flat(bb):
        return [b for row in bb for b in row]

    psall = nc.alloc_psum_tensor("psall", [128, 8, 512], F32)

    class BankV:
        def __init__(self, i):
            self.i = i

        def __getitem__(self, idx):
            if not isinstance(idx, tuple):
                idx = (idx,)
            return psall[(idx[0], self.i) + tuple(idx[1:])]

    banks = [BankV(i) for i in range(8)]
    B_bank = [Buf(f"bank{i}") for i in range(8)]

    ident_f = consts[:, C_ID:C_ID + 128]
    Dt = consts[:, C_D:C_D + 512]
    Dm = consts[:, C_DM:C_DM + 512]

    def mm(out_ap, lhsT, rhs, start, stop, reads, writes):
        return P.op("pe", lambda e: e.matmul(out_ap, lhsT, rhs, start=start, stop=stop), reads, writes)

    def tr(out_ap, in_ap, ident, reads, writes):
        return P.op("pe", lambda e: e.transpose(out_ap, in_ap, ident), reads, writes)

    def act(out_ap, in_ap, func, reads, writes, bias=None, scale=None, accum_out=None):
        kw = {}
        if bias is not None:
            kw["bias"] = bias
        if scale is not None:
            kw["scale"] = scale
        if accum_out is not None:
            kw["accum_out"] = accum_out
        return P.op("act", lambda e: e.activation(out_ap, in_ap, func, **kw), reads, writes)

    def tt(eng, out_ap, in0, in1, op, reads, writes):
        return P.op(eng, lambda e: e.tensor_tensor(out_ap, in0, in1, op), reads, writes)

    def ts(eng, out_ap, in0, s1, s2, op0, op1, reads, writes):
        if s2 is None:
            return P.op(eng, lambda e: e.tensor_scalar(out_ap, in0, s1, None, op0), reads, writes)
        return P.op(eng, lambda e: e.tensor_scalar(out_ap, in0, s1, s2, op0, op1), reads, writes)

    def stt(out_ap, in0, scalar, in1, op0, op1, reads, writes):
        return P.op("dve", lambda e: e.scalar_tensor_tensor(out_ap, in0, scalar, in1, op0, op1), reads, writes)

    def cp(eng, out_ap, in_ap, reads, writes):
        if eng == "act":
            return P.op("act", lambda e: e.activation(out_ap, in_ap, AF.Copy), reads, writes)
        return P.op(eng, lambda e: e.tensor_copy(out_ap, in_ap), reads, writes)

    def red(out_ap, in_ap, op, reads, writes):
        return P.op("dve", lambda e: e.tensor_reduce(out_ap, in_ap, AX.X, op), reads, writes)

    def recip(out_ap, in_ap, reads, writes):
        return P.op("dve", lambda e: e.reciprocal(out_ap, in_ap), reads, writes)

    def dma(q, out_ap, in_ap, reads, writes, gid=None, exempt=False, noncontig=False):
        if noncontig:
            return P.dma(q, lambda e: e.dma_start(out=out_ap, in_=in_ap, allow_slow_non_contiguous=True), reads, writes, gid, exempt)
        return P.dma(q, lambda e: e.dma_start(out=out_ap, in_=in_ap), reads, writes, gid, exempt)

    BUFS = {}

    def GB(name):
        if name not in BUFS:
            BUFS[name] = Buf(name)
        return BUFS[name]

    gidc = [0]

    def newgid():
        gidc[0] += 1
        return gidc[0]

    dma("sp", consts[:], consts_in[:, :], [], [B_consts])
    dma("sp", vecs[:], vecs_in[:, :], [], [B_vecs])
    dma("sp", wr_s[:], wr_in[:, :, :, :], [], [B_wr])
    dma("sp", rb_s[:], rb_in[:, :], [], [B_rb])
    dma("sp", cact[:], cT_in[:, :, :], [], [B_cact])
    dma("sp", subg[:], subg_in[:, :], [], [B_subg])
    cp("dve", ident_b[:], ident_f, [B_consts], [B_identb])
    P.op("dve", lambda e: e.memset(ones_b[:], 1.0), [], [B_onesb])
    P.op("dve", lambda e: e.memset(ones_f[:], 1.0), [], [B_onesf])
    for j in range(NCH):
        P.op("dve", lambda e, j=j: e.memset(zhist[:, j, :], 0.0), [], [B_zh[j]])
    act(cact[:], cact[:], AF.Silu, [B_cact], [B_cact])

    lam_s = salloc("lam_s", [128, 2, 4, 64], F32, at=U0)
    lam_t = salloc("lam_t", [128, 2, 2, 64], F32, at=U0 + 2048)
    lam_r = salloc("lam_r", [128, 2, 2], F32, at=U0 + 3072)
    B_lam = Buf("lam")
    dma("sp", lam_s[:], lam_in[:, :].rearrange("p (l k d) -> p l k d", l=2, k=4), [], [B_lam])
    for j in range(2):
        tt("dve", lam_t[:, j, 0, :], lam_s[:, j, 0, :], lam_s[:, j, 1, :], ALU.mult, [B_lam], [B_lam])
        tt("dve", lam_t[:, j, 1, :], lam_s[:, j, 2, :], lam_s[:, j, 3, :], ALU.mult, [B_lam], [B_lam])
        red(lam_r[:, j, :], lam_t[:, j, :, :], ALU.add, [B_lam], [B_lam])
        act(lam_r[:, j, :], lam_r[:, j, :], AF.Exp, [B_lam], [B_lam])
        lam_init = 0.8 - 0.6 * float(np.exp(-0.3 * (j + N_A)))
        stt(neglam[:, j:j + 1], lam_r[:, j, 1:2], -lam_init, lam_r[:, j, 0:1], ALU.add, ALU.subtract,
            [B_lam], [B_neglam])

    stage = [salloc(f"stage{i}", [128, NCH, 512], F32, at=U0 + 8192 + i * 16384) for i in range(2)]
    B_stage = [Buf("stage0"), Buf("stage1")]
    pieces = []
    for l in range(DEPTH):
        for pc in range(12):
            pieces.append((mod_w[l], pc, l * 48 + pc * 4, V_MODB + l * 48 + pc * 4))
    for pc in range(4):
        pieces.append((kv_mod_w, pc, 192 + pc * 4, V_KVB + pc * 4))
    for i, (src, pc, col0, vcol0) in enumerate(pieces):
        sb, st_ = B_stage[i % 2], stage[i % 2]
        dma("sp", st_[:], src[:, pc * 512:(pc + 1) * 512].rearrange("(kc p) n -> p kc n", p=128), [], [sb])
        bk = i % 2
        for jj in range(4):
            for kc in range(NCH):
                mm(banks[bk][:, jj * 2:jj * 2 + 2], st_[:, kc, jj * 128:(jj + 1) * 128], cact[:, kc, :],
                   kc == 0, kc == NCH - 1, [sb, B_cact], [B_bank[bk]])
        for jj in range(4):
            ts("dve", modT[:, col0 + jj, :], banks[bk][:, jj * 2:jj * 2 + 2], vecs[:, vcol0 + jj:vcol0 + jj + 1], None,
               ALU.add, None, [B_bank[bk], B_vecs], [B_modT])

    def mcol(l, which):
        return l * 48 + which * 8

    for l in range(DEPTH):
        for (slot, which, vg) in ((l, 1, V_GMIX + l * 8), (4 + l, 4, V_GFFN + l * 8)):
            for b in range(NB_CORE):
                stt(gm[:, slot, :, b], modT[:, mcol(l, which):mcol(l, which) + 8, b], 1.0, vecs[:, vg:vg + 8],
                    ALU.add, ALU.mult, [B_modT, B_vecs], [B_gm])
    for b in range(NB_CORE):
        stt(gm[:, 8, :, b], modT[:, 192 + 8:192 + 16, b], 1.0, vecs[:, V_GKV:V_GKV + 8], ALU.add, ALU.mult,
            [B_modT, B_vecs], [B_gm])
    bias_vals = {}
    B_bias = Buf("bias")
    for hd_ in range(NH):
        for kk in range(0, 32):
            v_ = float(np.float32(-SLOPES[hd_] * 128.0 * kk))
            if v_ not in bias_vals:
                k_ = len(bias_vals)
                assert k_ < 256
                bias_vals[v_] = k_
                P.op("dve", lambda e, k_=k_, v_=v_: e.memset(bias_sb[:, k_:k_ + 1], v_), [], [B_bias])
    P.barrier()

    def norm_stats():
        for h in range(2):
            for c in range(NCH):
                act(hT[:, c, h * 512:(h + 1) * 512], xT[:, c, h * 512:(h + 1) * 512], AF.Square,
                    [B_xT[c][h]], [B_hT[c][h]])
        for h in range(2):
            for c in range(NCH):
                mm(banks[6 + h][:, :], ones_b[:, :], hT[:, c, h * 512:(h + 1) * 512], c == 0, c == NCH - 1,
                   [B_onesb, B_hT[c][h]], [B_bank[6 + h]])
        for h in range(2):
            act(rstd[:, h * 512:(h + 1) * 512], banks[6 + h][:, :], AF.Sqrt, [B_bank[6 + h]], [B_rstd[h]],
                bias=eps_ap, scale=1.0 / D)
            recip(rstd[:, h * 512:(h + 1) * 512], rstd[:, h * 512:(h + 1) * 512], [B_rstd[h]], [B_rstd[h]])

    B_eps = Buf("eps")
    P.op("dve", lambda e: e.memset(eps_t[:], EPS), [], [B_eps])
    eps_ap = eps_t[:, 0:1]

    def norm_mod(gslot, shcol, b, h32=None, B_h32=None, bar_after_stats=False):
        norm_stats()
        if bar_after_stats:
            P.barrier()
        k = 0
        for h in range(2):
            for c in range(NCH):
                sl = slice(h * 512, (h + 1) * 512)
                if h32 is None:
                    ti = k % 2
                    k += 1
                    stt(tmp32[:, ti, :], xT[:, c, sl], gm[:, gslot, c, b:b + 1], rstd[:, sl], ALU.mult,
                        ALU.mult, [B_xT[c][h], B_gm, B_rstd[h], B_eps], [B_tmp32[ti]])
                    act(hT[:, c, sl], tmp32[:, ti, :], AF.Identity,
                        [B_tmp32[ti], B_modT], [B_hT[c][h]],
                        bias=modT[:, shcol + c, b:b + 1], scale=1.0)
                else:
                    stt(h32[:, c, sl], xT[:, c, sl], gm[:, gslot, c, b:b + 1], rstd[:, sl], ALU.mult, ALU.mult,
                        [B_xT[c][h], B_gm, B_rstd[h], B_eps], [B_h32[c][h]])
                    act(h32[:, c, sl], h32[:, c, sl], AF.Identity, [B_h32[c][h], B_modT], [B_h32[c][h]],
                        bias=modT[:, shcol + c, b:b + 1], scale=1.0)
                    cp("dve" if (c + h) % 2 else "act", hT[:, c, sl], h32[:, c, sl], [B_h32[c][h]], [B_hT[c][h]])

    B_tmp32 = [Buf("tmp32a"), Buf("tmp32b")]

    def load_x(l, st):
        if l == 0:
            xtok = [salloc(f"xtok{i}_{st}", [128, D], F32, at=U0 + i * 4096) for i in range(2)]
            B_xtok = [GB("xtok0"), GB("xtok1")]
            for t8 in range(8):
                i = t8 % 2
                r0 = st * TS + t8 * 128
                dma("sp", xtok[i][:], x_in[r0:r0 + 128, :], [], [B_xtok[i]])
                for c4 in range(2):
                    bk = (t8 * 2 + c4) % 2
                    for cc in range(4):
                        c = c4 * 4 + cc
                        tr(banks[bk][:, cc * 128:(cc + 1) * 128], xtok[i][:, c * 128:(c + 1) * 128], ident_f,
                           [B_xtok[i], B_consts], [B_bank[bk]])
                    h = t8 // 4
                    cp("act" if c4 else "dve", xT[:, c4 * 4:c4 * 4 + 4, t8 * 128:(t8 + 1) * 128],
                       banks[bk][:, :].rearrange("p (c t) -> p c t", c=4), [B_bank[bk]],
                       [B_xT[c4 * 4 + cc][h] for cc in range(4)])
        else:
            g = newgid()
            for c in range(NCH):
                dma("sp", xT[:, c, :], xT_d[:, c, st * TS:(st + 1) * TS], [xd_bufs[st]], [B_xT[c][0], B_xT[c][1]], gid=g)

    def store_x(st):
        g = newgid()
        for c in range(NCH):
            dma("sp", xT_d[:, c, st * TS:(st + 1) * TS], xT[:, c, :], [B_xT[c][0], B_xT[c][1]], [xd_bufs[st]], gid=g)


    vcnt = [0]

    def rview(i, shape):
        vcnt[0] += 1
        assert int(np.prod(shape[1:])) * 2 + (i * 8192) <= 6 * 8192
        return nc.alloc_sbuf_tensor_at(f"rv{vcnt[0]}", list(shape), BF16, offset=ring_base + i * 8192)

    def uview(name, shape, dt, off):
        vcnt[0] += 1
        nbytes = int(np.prod(shape[1:])) * (4 if dt == F32 else 2)
        assert U0 + off + nbytes <= 229344, (name, off, nbytes, U0)
        return nc.alloc_sbuf_tensor_at(f"{name}_{vcnt[0]}", list(shape), dt, offset=U0 + off)

    def wload(pieces_idx, dst_ap, src_ap):
        return dma("pool", dst_ap, src_ap, [], [B_ring[i] for i in pieces_idx], exempt=True)

    def conv_mixer(l, st, b):
        first = (st % 4 == 0)
        zb = uview("zb", [128, NCH, TS], BF16, 0)
        z = uview("z", [128, TS + 2], F32, 16384)
        zc = uview("zc", [128, TS], F32, 16384 + 4128)
        cs = uview("cs", [128, 2, 512], F32, 16384 + 4128 + 4096)
        B_zb = [[GB(f"zb{c}_{h}") for h in range(2)] for c in range(NCH)]
        B_z, B_zc, B_cs = GB("z"), GB("zc"), [GB("cs0"), GB("cs1")]
        norm_mod(l, mcol(l, 0), b)
        P.barrier()
        wsrc = conv_in_w[l].rearrange("(kc p) (g n) -> p kc g n", p=128, g=3)
        wout = rview(4, [128, NCH, D])
        wload([4, 5], wout[:], conv_out_w[l].rearrange("(kc p) n -> p kc n", p=128))
        for j in range(NCH):
            pi = j % 2
            wj = rview(pi, [128, NCH, 3, 128])
            g_ = newgid()
            for g3 in range(3):
                dma("pool", wj[:, :, g3, :], wsrc[:, :, g3, j * 128:(j + 1) * 128], [], [B_ring[pi]], gid=g_, exempt=True)
            if first:
                P.op("dve", lambda e: e.memset(z[:, 0:2], 0.0), [], [B_z])
            else:
                cp("dve", z[:, 0:2], zhist[:, j, :], [B_zh[j]], [B_z])
            for h in range(2):
                sl = slice(h * 512, (h + 1) * 512)
                bb = 3 * h
                for g in (1, 2):
                    for kc in range(NCH):
                        mm(banks[bb + g][:, :], wj[:, kc, g, :], hT[:, kc, sl], kc == 0, kc == NCH - 1,
                           [B_ring[pi], B_hT[kc][h]], [B_bank[bb + g]])
                cp("act", cs[:, h, :], banks[bb + 1][:, :], [B_bank[bb + 1]], [B_cs[h]])
                tt("dve", z[:, 2 + h * 512:2 + (h + 1) * 512], cs[:, h, :], banks[bb + 2][:, :], ALU.mult,
                   [B_cs[h], B_bank[bb + 2]], [B_z])
            for h in range(2):
                sl = slice(h * 512, (h + 1) * 512)
                bb = 3 * h
                for kc in range(NCH):
                    mm(banks[bb][:, :], wj[:, kc, 0, :], hT[:, kc, sl], kc == 0, kc == NCH - 1,
                       [B_ring[pi], B_hT[kc][h]], [B_bank[bb]])
            cw = V_CW + (l * 8 + j) * 3
            ts("dve", zc[:, :], z[:, 2:2 + TS], vecs[:, cw + 2:cw + 3], vecs[:, V_CB + l * 8 + j:V_CB + l * 8 + j + 1],
               ALU.mult, ALU.add, [B_z, B_vecs], [B_zc])
            stt(zc[:, :], z[:, 1:1 + TS], vecs[:, cw + 1:cw + 2], zc[:, :], ALU.mult, ALU.add, [B_z, B_vecs, B_zc], [B_zc])
            stt(zc[:, :], z[:, 0:TS], vecs[:, cw:cw + 1], zc[:, :], ALU.mult, ALU.add, [B_z, B_vecs, B_zc], [B_zc])
            cp("dve", zhist[:, j, :], z[:, TS:TS + 2], [B_z], [B_zh[j]])
            for h in range(2):
                sl = slice(h * 512, (h + 1) * 512)
                tt("dve", zb[:, j, sl], zc[:, sl], banks[3 * h][:, :], ALU.mult, [B_zc, B_bank[3 * h]], [B_zb[j][h]])
        gcol = mcol(l, 2)
        for oc in range(NCH):
            for h in range(2):
                sl = slice(h * 512, (h + 1) * 512)
                bk = 6 + (oc * 2 + h) % 2
                for kc in range(NCH):
                    mm(banks[bk][:, :], wout[:, kc, oc * 128:(oc + 1) * 128], zb[:, kc, sl], kc == 0, kc == NCH - 1,
                       [B_ring[4], B_ring[5], B_zb[kc][h]], [B_bank[bk]])
                stt(xT[:, oc, sl], banks[bk][:, :], modT[:, gcol + oc, b:b + 1], xT[:, oc, sl], ALU.mult, ALU.add,
                    [B_bank[bk], B_modT, B_xT[oc][h]], [B_xT[oc][h]])

    def moe(l, st, b):
        h32 = uview("h32", [128, NCH, TS], F32, 0)
        acc = uview("acc", [128, NCH, TS], F32, 32768)
        hid = uview("hid", [128, 2, 4, 512], BF16, 65536)
        sa = uview("sa", [128, 2, 512], F32, 65536 + 8192)
        o = 65536 + 8192 + 4096
        Ls = uview("Ls", [128, 8, 20], F32, o); o += 640
        r4 = [uview(f"r4{i}", [128, 8, 4], F32, o + i * 128) for i in range(8)]; o += 8 * 128
        r1 = [uview(f"r1{i}", [128, 8], F32, o + i * 32) for i in range(8)]; o += 8 * 32
        t16 = uview("t16", [128, 8, 4, 4], F32, o); o += 512
        comb = uview("comb", [128, 8, 4, 4], F32, o); o += 512
        B_h32 = [[GB(f"h32_{c}_{h}") for h in range(2)] for c in range(NCH)]
        B_acc = [[GB(f"acc_{c}_{h}") for h in range(2)] for c in range(NCH)]
        B_hid = [[GB(f"hid_{h}_{m}") for m in range(4)] for h in range(2)]
        B_sa = [GB("sa0"), GB("sa1")]
        B_r = GB("router")
        norm_mod(4 + l, mcol(l, 3), b, h32, B_h32, bar_after_stats=True)
        LB = banks[7][:, 0:256].rearrange("p (t n) -> p t n", t=8)
        for t8 in range(8):
            h = t8 // 4
            for kc in range(NCH):
                mm(LB[:, t8, 0:20], h32[:, kc, t8 * 128:(t8 + 1) * 128], wr_s[:, l, kc, :], kc == 0, False,
                   [B_h32[kc][h], B_wr], [B_bank[7]])
            mm(LB[:, t8, 0:20], ones_f[0:1, :], rb_s[0:1, l * 20:(l + 1) * 20], False, True, [B_onesf, B_rb], [B_bank[7]])
        cp("dve", Ls[:, :, :], LB[:, :, 0:20], [B_bank[7]], [B_r])
        R = [B_r]
        gl = Ls[:, :, 0:4]
        gmax, gs, pg, m1, m2, w1, w2 = r1[0], r1[1], r1[2], r1[3], r1[4], r1[5], r1[6]
        gsh, oh, esel, d1, mk1, e2, mk2, wexp = r4

        def bc4(a):
            return a[:, :].unsqueeze(2).to_broadcast([128, 8, 4])

        red(gmax[:, :], gl, ALU.max, R, R)
        tt("dve", gsh[:, :, :], gl, bc4(gmax), ALU.subtract, R, R)
        ts("dve", oh[:, :, :], gsh[:, :, :], 0.0, None, ALU.is_equal, None, R, R)
        act(gsh[:, :, :], gsh[:, :, :], AF.Exp, R, R)
        red(gs[:, :], gsh[:, :, :], ALU.add, R, R)
        recip(pg[:, :], gs[:, :], R, R)
        el = Ls[:, :, 4:20].rearrange("p t (g e) -> p t g e", g=4)
        tt("dve", t16[:, :, :, :], el, oh[:, :, :].unsqueeze(3).to_broadcast([128, 8, 4, 4]), ALU.mult, R, R)
        tt("dve", esel[:, :, :], t16[:, :, 0, :], t16[:, :, 1, :], ALU.add, R, R)
        tt("dve", esel[:, :, :], esel[:, :, :], t16[:, :, 2, :], ALU.add, R, R)
        tt("dve", esel[:, :, :], esel[:, :, :], t16[:, :, 3, :], ALU.add, R, R)
        red(m1[:, :], esel[:, :, :], ALU.max, R, R)
        tt("dve", d1[:, :, :], esel[:, :, :], bc4(m1), ALU.subtract, R, R)
        ts("dve", mk1[:, :, :], d1[:, :, :], 0.0, None, ALU.is_equal, None, R, R)
        stt(e2[:, :, :], mk1[:, :, :], -1e30, d1[:, :, :], ALU.mult, ALU.add, R, R)
        red(m2[:, :], e2[:, :, :], ALU.max, R, R)
        tt("dve", d1[:, :, :], e2[:, :, :], bc4(m2), ALU.subtract, R, R)
        ts("dve", mk2[:, :, :], d1[:, :, :], 0.0, None, ALU.is_equal, None, R, R)
        act(w2[:, :], m2[:, :], AF.Exp, R, R)
        ts("dve", w1[:, :], w2[:, :], 1.0, None, ALU.add, None, R, R)
        recip(w1[:, :], w1[:, :], R, R)
        tt("dve", w2[:, :], w2[:, :], w1[:, :], ALU.mult, R, R)
        tt("dve", w1[:, :], w1[:, :], pg[:, :], ALU.mult, R, R)
        tt("dve", w2[:, :], w2[:, :], pg[:, :], ALU.mult, R, R)
        tt("dve", wexp[:, :, :], mk1[:, :, :], bc4(w1), ALU.mult, R, R)
        tt("dve", mk2[:, :, :], mk2[:, :, :], bc4(w2), ALU.mult, R, R)
        tt("dve", wexp[:, :, :], wexp[:, :, :], mk2[:, :, :], ALU.add, R, R)
        tt("dve", comb[:, :, :, :], oh[:, :, :].unsqueeze(3).to_broadcast([128, 8, 4, 4]),
           wexp[:, :, :].unsqueeze(2).to_broadcast([128, 8, 4, 4]), ALU.mult, R, R)
        combf = comb[:, :, :, :].rearrange("p t g e -> p t (g e)")

        wviews = {}

        def S1(u, e, h):
            s3 = 3 * (e % 2)
            if h == 0:
                w1v = rview(s3, [128, NCH, DE])
                w3v = rview(s3 + 1, [128, NCH, DE])
                w2v = rview(s3 + 2, [128, 4, D])
                wload([s3], w1v[:], exp_w1[l, e].rearrange("(kc p) n -> p kc n", p=128))
                wload([s3 + 1], w3v[:], exp_w3[l, e].rearrange("(kc p) n -> p kc n", p=128))
                wload([s3 + 2], w2v[:], exp_w2[l, e].rearrange("(kc p) n -> p kc n", p=128))
                wviews[e] = (w1v, w3v, w2v)
            w1v, w3v, w2v = wviews[e]
            sl = slice(h * 512, (h + 1) * 512)
            bbk = 4 if u % 2 == 0 else 7

            def hid_op(m):
                tt("dve", hid[:, h, m, :], sa[:, m % 2, :], banks[bbk][:, :], ALU.mult, [B_sa[m % 2], B_bank[bbk]],
                   [B_hid[h][m]])

            for m in range(4):
                ba, bb_ = 2 * (m % 2), 2 * (m % 2) + 1
                for kc in range(NCH):
                    mm(banks[ba][:, :], w1v[:, kc, m * 128:(m + 1) * 128], hT[:, kc, sl], kc == 0, kc == NCH - 1,
                       [B_ring[s3], B_hT[kc][h]], [B_bank[ba]])
                for kc in range(NCH):
                    mm(banks[bb_][:, :], w3v[:, kc, m * 128:(m + 1) * 128], hT[:, kc, sl], kc == 0, kc == NCH - 1,
                       [B_ring[s3 + 1], B_hT[kc][h]], [B_bank[bb_]])
                if m == 1:
                    for t4 in range(4):
                        t8 = h * 4 + t4
                        mm(banks[bbk][:, t4 * 128:(t4 + 1) * 128], combf[:, t8, e:e + 1].to_broadcast([128, 128]),
                           ident_f, True, True, [B_r, B_consts], [B_bank[bbk]])
                    hid_op(0)
                act(sa[:, m % 2, :], banks[ba][:, :], AF.Silu, [B_bank[ba]], [B_sa[m % 2]])
                tt("dve", sa[:, m % 2, :], sa[:, m % 2, :], banks[bb_][:, :], ALU.mult, [B_sa[m % 2], B_bank[bb_]],
                   [B_sa[m % 2]])
                if m >= 1:
                    hid_op(m)

        def S2(u, e, h):
            s3 = 3 * (e % 2)
            w1v, w3v, w2v = wviews[e]
            sl = slice(h * 512, (h + 1) * 512)
            for j in range(NCH):
                bo = 5 + (j % 2)
                for m in range(4):
                    mm(banks[bo][:, :], w2v[:, m, j * 128:(j + 1) * 128], hid[:, h, m, :], m == 0, m == 3,
                       [B_ring[s3 + 2], B_hid[h][m]], [B_bank[bo]])
                if e == 0:
                    cp("act", acc[:, j, sl], banks[bo][:, :], [B_bank[bo]], [B_acc[j][h]])
                else:
                    tt("dve", acc[:, j, sl], acc[:, j, sl], banks[bo][:, :], ALU.add, [B_acc[j][h], B_bank[bo]],
                       [B_acc[j][h]])

        units = [(e, h) for e in range(NE) for h in range(2)]
        for u, (e, h) in enumerate(units):
            S1(u, e, h)
            if u >= 1:
                S2(u - 1, *units[u - 1])
        S2(len(units) - 1, *units[-1])
        gcol = mcol(l, 5)
        for j in range(NCH):
            for h in range(2):
                sl = slice(h * 512, (h + 1) * 512)
                stt(xT[:, j, sl], acc[:, j, sl], modT[:, gcol + j, b:b + 1], xT[:, j, sl], ALU.mult, ALU.add,
                    [B_acc[j][h], B_modT, B_xT[j][h]], [B_xT[j][h]])

    def kv_stage(st, b):
        kst = uview("kst", [128, NH, TS], BF16, 0)
        vst = uview("vst", [128, 8, D], BF16, 16384)
        B_kst = [GB(f"kst{i}") for i in range(NH)]
        B_vst = [GB(f"vst{i}") for i in range(8)]
        norm_mod(8, 192, b)
        wk = rview(0, [128, NCH, D])
        wv = rview(2, [128, NCH, D])
        wload([0, 1], wk[:], kv_w[:, 0:D].rearrange("(kc p) n -> p kc n", p=128))
        wload([2, 3], wv[:], kv_w[:, D:2 * D].rearrange("(kc p) n -> p kc n", p=128))
        n = 0
        for hd in range(NH):
            for h in range(2):
                sl = slice(h * 512, (h + 1) * 512)
                bk = n % 4; n += 1
                for kc in range(NCH):
                    mm(banks[bk][:, :], wk[:, kc, hd * 128:(hd + 1) * 128], hT[:, kc, sl], kc == 0, kc == NCH - 1,
                       [B_ring[0], B_ring[1], B_hT[kc][h]], [B_bank[bk]])
                cp("act" if n % 2 else "dve", kst[:, hd, sl], banks[bk][:, :], [B_bank[bk]], [B_kst[hd]])
        g = newgid()
        for hd in range(NH):
            dma("sp", kT_d[:, hd, st * TS:(st + 1) * TS], kst[:, hd, :], [B_kst[hd]], [kd_bufs[st]], gid=g)
        for t8 in range(8):
            h = t8 // 4
            for nh in range(2):
                bk = n % 4; n += 1
                for kc in range(NCH):
                    mm(banks[bk][:, :], hT[:, kc, t8 * 128:(t8 + 1) * 128], wv[:, kc, nh * 512:(nh + 1) * 512], kc == 0,
                       kc == NCH - 1, [B_ring[2], B_ring[3], B_hT[kc][h]], [B_bank[bk]])
                cp("act" if n % 2 else "dve", vst[:, t8, nh * 512:(nh + 1) * 512], banks[bk][:, :], [B_bank[bk]],
                   [B_vst[t8]])
        for t8 in range(8):
            r0 = st * TS + t8 * 128
            dma("sp", v_d[r0:r0 + 128, :], vst[:, t8, :], [B_vst[t8]], [vd_bufs[st]], gid=g)

    def attn(l, st, b):
        ja = l - N_A
        lam_init = 0.8 - 0.6 * float(np.exp(-0.3 * l))
        sq = st % 4
        s0 = (st // 4) * 4
        nkeys = (sq + 1) * TS
        nkt_all = nkeys // 128
        NPB, NTB, LOOK = 4, 3, 2
        qT = uview("qT", [128, NH, TS], BF16, 0)
        kh = [uview(f"kh{i}", [128, SEQ], BF16, 16384 + i * 8192) for i in range(2)]
        vh = [uview(f"vh{i}", [128, 32, 128], BF16, 32768 + i * 8192) for i in range(2)]
        pb = [uview(f"pb{i}", [128, 2, 512], BF16, 49152 + i * 2048) for i in range(NPB)]
        o = 49152 + NPB * 2048
        tb = [uview(f"tb{i}", [128, 2, 512], F32, o + i * 4096) for i in range(NTB)]
        o += NTB * 4096
        r1 = uview("r1", [128, 512], F32, o); o += 2048
        r2 = uview("r2", [128, 512], F32, o); o += 2048
        e1 = uview("e1", [128, 512], F32, o); o += 2048
        e2 = uview("e2", [128, 512], F32, o); o += 2048
        sqb = uview("sqb", [128, 512], BF16, o); o += 1024
        subs = uview("subs", [128, 1], F32, o); o += 32
        B_q = [[GB(f"q{a}_{h}") for h in range(2)] for a in range(NH)]
        B_kh, B_vh = [GB("kh0"), GB("kh1")], [GB("vh0"), GB("vh1")]
        B_pb, B_tb = [GB(f"pb{i}") for i in range(NPB)], [GB(f"tb{i}") for i in range(NTB)]
        B_r1, B_r2, B_e1, B_e2, B_sqb, B_subs = (GB(n) for n in ("ar1", "ar2", "ae1", "ae2", "asqb", "asubs"))
        norm_mod(l, mcol(l, 0), b)
        P.barrier()
        wq = rview(0, [128, NCH, D])
        wload([0, 1], wq[:], q_w[ja].rearrange("(kc p) n -> p kc n", p=128))
        wo = rview(2, [128, NCH, D])
        wload([2, 3], wo[:], o_w[ja].rearrange("(kc p) n -> p kc n", p=128))
        ts("dve", subs[:, :], subg[:, ja:ja + 1], 1.0 - lam_init, None, ALU.mult, None, [B_subg], [B_subs])

        def load_kv(hd):
            ki = hd % 2
            tk0 = s0 * TS
            dma("sp", kh[ki][:, 0:nkeys], kT_d[:, hd, tk0:tk0 + nkeys], kd_bufs[s0:s0 + sq + 1], [B_kh[ki]])
            dma("sp", vh[ki][:, 0:nkt_all, :],
                v_d[tk0:tk0 + nkeys, hd * 128:(hd + 1) * 128].rearrange("(kt p) d -> p kt d", p=128),
                vd_bufs[s0:s0 + sq + 1], [B_vh[ki]])

        load_kv(0)
        load_kv(1)
        n = 0
        for hd in range(NH):
            for h in range(2):
                sl = slice(h * 512, (h + 1) * 512)
                bk = n % 2; n += 1
                for kc in range(NCH):
                    mm(banks[bk][:, :], wq[:, kc, hd * 128:(hd + 1) * 128], hT[:, kc, sl], kc == 0, kc == NCH - 1,
                       [B_ring[0], B_ring[1], B_hT[kc][h]], [B_bank[bk]])
                cp("act" if n % 2 else "dve", qT[:, hd, sl], banks[bk][:, :], [B_bank[bk]], [B_q[hd][h]])
        tiles = []
        for hd in range(NH):
            for h in range(2):
                q0 = sq * TS + h * 512
                nkt = q0 // 128 + 4
                for kt in range(nkt):
                    tiles.append((hd, h, kt, q0, nkt))

        def front(n, t):
            hd, h, kt, q0, nkt = t
            ki = hd % 2
            slope = SLOPES[hd]
            jd = kt - q0 // 128
            qoff = 128 * jd if jd > 0 else 0
            nn = 512 - qoff
            sp, pi, ti = 2 * (n % 2), n % NPB, n % NTB
            for s in range(2):
                ps = slice(s * 64, (s + 1) * 64)
                mm(banks[sp + s][:, 0:nn], kh[ki][ps, kt * 128:(kt + 1) * 128],
                   qT[ps, hd, h * 512 + qoff:(h + 1) * 512], True, True, [B_kh[ki], B_q[hd][h]], [B_bank[sp + s]])
            dsel = Dm if jd >= 0 else Dt
            stt(tb[ti][:, :, 0:nn], dsel[:, 0:nn].unsqueeze(1).to_broadcast([128, 2, nn]), -slope * 8.0,
                psall[:, sp:sp + 2, 0:nn], ALU.mult, ALU.add, [B_consts, B_bank[sp], B_bank[sp + 1]], [B_tb[ti]])
            cb = -slope * float(q0 - kt * 128) if jd < 0 else 0.0
            act(pb[pi][:, :, qoff:512], tb[ti][:, :, 0:nn], AF.Exp, [B_tb[ti], B_bias], [B_pb[pi]],
                bias=bias_tile(cb), scale=0.125)

        def back(n, t):
            hd, h, kt, q0, nkt = t
            ki = hd % 2
            jd = kt - q0 // 128
            qoff = 128 * jd if jd > 0 else 0
            pi = n % NPB
            first, last = (kt == 0), (kt == nkt - 1)
            for s in range(2):
                mm(banks[4 + s][:, qoff:512], vh[ki][:, kt, :], pb[pi][:, s, qoff:512], first, last,
                   [B_vh[ki], B_pb[pi]], [B_bank[4 + s]])
            for s in range(2):
                mm(banks[6 + s][:, qoff:512], ones_b[:, :], pb[pi][:, s, qoff:512], first, last,
                   [B_onesb, B_pb[pi]], [B_bank[6 + s]])
            if not last:
                return
            act(r1[:, :], banks[6][:, :], AF.Ln, [B_bank[6]], [B_r1])
            act(r1[:, :], r1[:, :], AF.Exp, [B_r1], [B_r1], scale=-1.0)
            act(r2[:, :], banks[7][:, :], AF.Ln, [B_bank[7]], [B_r2])
            act(r2[:, :], r2[:, :], AF.Exp, [B_r2], [B_r2], scale=-1.0)
            tt("dve", e1[:, :], banks[4][:, :], r1[:, :], ALU.mult, [B_bank[4], B_r1], [B_e1])
            stt(e2[:, :], banks[5][:, :], neglam[:, ja:ja + 1], r2[:, :], ALU.mult, ALU.mult,
                [B_bank[5], B_neglam, B_r2], [B_e2])
            tt("pool", e1[:, :], e1[:, :], e2[:, :], ALU.add, [B_e1, B_e2], [B_e1])
            tt("pool", sqb[:, :], e1[:, :], e1[:, :], ALU.mult, [B_e1], [B_sqb])
            pending.append((n + LOOK + DELAY, hd, h))
            if h == 1 and hd + 2 < NH:
                load_kv(hd + 2)

        def evac2(hd, h, bk):
            sl = slice(h * 512, (h + 1) * 512)
            mm(banks[bk][:, :], ones_b[:, :], sqb[:, :], True, True, [B_onesb, B_sqb], [B_bank[bk]])
            act(r1[:, :], banks[bk][:, :], AF.Ln, [B_bank[bk], B_eps], [B_r1], bias=eps_ap, scale=1.0 / 128)
            act(r1[:, :], r1[:, :], AF.Exp, [B_r1], [B_r1], scale=-0.5)
            stt(hT[:, hd, sl], e1[:, :], subs[:, 0:1], r1[:, :], ALU.mult, ALU.mult, [B_e1, B_subs, B_r1],
                [B_hT[hd][h]])

        DELAY = 2
        pending = []
        for n, t in enumerate(tiles):
            front(n, t)
            if n >= LOOK:
                back(n - LOOK, tiles[n - LOOK])
            while pending and pending[0][0] <= n:
                _, hd_, h_ = pending.pop(0)
                evac2(hd_, h_, 2 * ((n + 1) % 2))
        for n in range(max(len(tiles) - LOOK, 0), len(tiles)):
            back(n, tiles[n])
        for (_, hd_, h_) in pending:
            evac2(hd_, h_, 0)
        gcol = mcol(l, 2)
        for oc in range(NCH):
            for h in range(2):
                sl = slice(h * 512, (h + 1) * 512)
                bk = (oc * 2 + h) % 2
                for kc in range(NCH):
                    mm(banks[bk][:, :], wo[:, kc, oc * 128:(oc + 1) * 128], hT[:, kc, sl], kc == 0, kc == NCH - 1,
                       [B_ring[2], B_ring[3], B_hT[kc][h]], [B_bank[bk]])
                stt(xT[:, oc, sl], banks[bk][:, :], modT[:, gcol + oc, b:b + 1], xT[:, oc, sl], ALU.mult, ALU.add,
                    [B_bank[bk], B_modT, B_xT[oc][h]], [B_xT[oc][h]])

    def bias_tile(v):
        k = bias_vals[float(np.float32(v))]
        return bias_sb[:, k:k + 1]

    def final_out(st, raw):
        y32 = uview("y32", [128, NCH, TS], F32, 0)
        otok = [uview(f"otok{i}", [128, D], F32, 32768 + i * 4096) for i in range(2)]
        B_y = [[GB(f"y{c}_{h}") for h in range(2)] for c in range(NCH)]
        B_ot = [GB("ot0"), GB("ot1")]
        if not raw:
            norm_stats()
        for c in range(NCH):
            for h in range(2):
                sl = slice(h * 512, (h + 1) * 512)
                if raw:
                    cp("dve", y32[:, c, sl], xT[:, c, sl], [B_xT[c][h]], [B_y[c][h]])
                else:
                    stt(y32[:, c, sl], xT[:, c, sl], vecs[:, V_GFIN + c:V_GFIN + c + 1], rstd[:, sl], ALU.mult, ALU.mult,
                        [B_xT[c][h], B_vecs, B_rstd[h]], [B_y[c][h]])
        g = newgid()
        n = 0
        for t8 in range(8):
            h = t8 // 4
            i = t8 % 2
            for c4 in range(2):
                bk = n % 2; n += 1
                for cc in range(4):
                    c = c4 * 4 + cc
                    tr(banks[bk][:, cc * 128:(cc + 1) * 128], y32[:, c, t8 * 128:(t8 + 1) * 128], ident_f,
                       [B_y[c][h], B_consts], [B_bank[bk]])
                cp("act" if c4 else "dve", otok[i][:, c4 * 512:(c4 + 1) * 512], banks[bk][:, :], [B_bank[bk]], [B_ot[i]])
            r0 = st * TS + t8 * 128
            dma("sp", out[r0:r0 + 128, :], otok[i][:], [B_ot[i]], [out_bufs[st]], gid=g)

    for l in range(n_layers):
        for st in range(NST):
            b = st // 4
            if l == 0:
                P.barrier()
            load_x(l, st)
            if l < N_A:
                conv_mixer(l, st, b)
            else:
                attn(l, st, b)
            moe(l, st, b)
            if l == N_A - 1 and n_layers > N_A:
                P.barrier()
                kv_stage(st, b)
            if l == n_layers - 1:
                P.barrier()
                final_out(st, raw=(debug_raw or n_layers < DEPTH))
            else:
                store_x(st)
    finals = [bf.w for bf in out_bufs if bf.w is not None]
    P.emit(finals)
    return nc, P


def _fm(v):
    v = np.asarray(v, np.float32).reshape(-1, 128)
    return np.ascontiguousarray(v.T)


def _host_layout(inputs, core):
    f32 = np.float32
    b0 = core * NB_CORE
    m = {}
    m["x"] = np.ascontiguousarray(inputs["x"][b0:b0 + NB_CORE].reshape(TOK, D))
    c2 = np.asarray(inputs["c"][b0:b0 + NB_CORE], f32)
    m["cT"] = np.ascontiguousarray(c2.T.reshape(NCH, 128, NB_CORE).transpose(1, 0, 2))
    vec = np.zeros((128, NV), f32)
    vec[:, V_MODB:V_MODB + 192] = _fm(inputs["mod_b"])
    vec[:, V_KVB:V_KVB + 16] = _fm(inputs["kv_mod_b"])
    vec[:, V_GMIX:V_GMIX + 32] = _fm(inputs["norm_mix_g"])
    vec[:, V_GFFN:V_GFFN + 32] = _fm(inputs["norm_ffn_g"])
    vec[:, V_GKV:V_GKV + 8] = _fm(inputs["kv_norm_g"])
    vec[:, V_GFIN:V_GFIN + 8] = _fm(inputs["final_norm_g"])
    cw = np.asarray(inputs["conv_w"], f32)
    vec[:, V_CW:V_CW + 48] = cw.reshape(2, 3, NCH, 128).transpose(3, 0, 2, 1).reshape(128, 48)
    vec[:, V_CB:V_CB + 16] = _fm(inputs["conv_b"])
    m["vecs"] = vec
    k = np.arange(128)[:, None].astype(f32)
    q = np.arange(512)[None, :].astype(f32)
    cst = np.zeros((128, NCONST), f32)
    cst[:, C_ID:C_ID + 128] = np.eye(128, dtype=f32)
    cst[:, C_D:C_D + 512] = q - k
    dmk = (q - k).copy()
    dmk[(q - k) < 0] = 1e30
    cst[:, C_DM:C_DM + 512] = dmk
    m["consts"] = cst
    wr = np.concatenate([np.asarray(inputs["router_group_w"], f32), np.asarray(inputs["router_exp_w"], f32)], axis=-1)
    m["wr"] = np.ascontiguousarray(wr.reshape(DEPTH, NCH, 128, 20).transpose(2, 0, 1, 3))
    rb = np.concatenate([np.asarray(inputs["router_group_b"], f32), np.asarray(inputs["router_exp_b"], f32)], axis=-1)
    m["rb"] = np.ascontiguousarray(rb.reshape(1, DEPTH * 20))
    lam = np.stack([inputs["lam_q1"], inputs["lam_k1"], inputs["lam_q2"], inputs["lam_k2"]], axis=1)
    m["lamv"] = np.ascontiguousarray(np.broadcast_to(np.asarray(lam, f32).reshape(1, -1), (128, 512)))
    m["subgT"] = np.ascontiguousarray(np.asarray(inputs["subln_g"], f32).T)
    for kname in ("mod_w", "kv_mod_w", "conv_in_w", "conv_out_w", "kv_w", "q_w", "o_w", "exp_w1", "exp_w3", "exp_w2"):
        m[kname] = np.ascontiguousarray(np.asarray(inputs[kname], f32))
    return m


def kernel(**inputs):
    nc, _ = build_program()
    in_maps = [_host_layout(inputs, core) for core in range(8)]
    res = run_bass_kernel_spmd(nc, in_maps, core_ids=list(range(8)))
    outs = [np.asarray(r["out"], np.float32).reshape(NB_CORE, SEQ, D) for r in res.results]
    return np.concatenate(outs, axis=0)
```

```python
import numpy as np
import concourse.bass as bass
import concourse.mybir as mybir
from concourse.bass_utils import run_bass_kernel_spmd

F32 = mybir.dt.float32
BF16 = mybir.dt.bfloat16
AF = mybir.ActivationFunctionType
ALU = mybir.AluOpType
AX = mybir.AxisListType

D = 1024
NCH = 8
SEQ = 4096
NB_CORE = 2
TOK = NB_CORE * SEQ
TS = 1024
NST = TOK // TS
DEPTH = 4
N_A = 2
NH = 8
NE = 16
DE = 512
EPS = 1e-6
SLOPES = [2.0 ** (-8.0 * (h + 1) / NH) for h in range(NH)]

V_MODB = 0
V_KVB = V_MODB + 4 * 48
V_GMIX = V_KVB + 16
V_GFFN = V_GMIX + 32
V_GKV = V_GFFN + 32
V_GFIN = V_GKV + 8
V_CW = V_GFIN + 8
V_CB = V_CW + 48
NV = V_CB + 16

C_ID = 0
C_D = 128
C_DM = 128 + 512
NCONST = 128 + 1024


class Buf:
    __slots__ = ("name", "w", "r", "sem", "cnt")

    def __init__(self, name):
        self.name = name
        self.w = None
        self.r = {}
        self.sem = None
        self.cnt = 0


class Prog:
    ENGS = ("pe", "act", "dve", "pool", "sp")

    def __init__(self, nc):
        self.nc = nc
        self.ops = {e: [] for e in self.ENGS}
        self.semh = {}
        self.cnt = {}
        self.nsem = 0
        for e in ("pe", "act", "dve", "pool"):
            self._newsem(e)
        self.last = {}
        self.pend_bar = {e: [] for e in self.ENGS}
        self.dma_pending = []
        self.nops = 0

    def _alloc_sem(self, name):
        self.nsem += 1
        return self.nc.alloc_semaphore(f"s{self.nsem}_{name}")

    def _newsem(self, e):
        self.semh[e] = self._alloc_sem(e)
        self.cnt[e] = 0

    def _tick(self, e):
        if self.cnt[e] >= 30000:
            self._newsem(e)
        self.cnt[e] += 1
        tok = (self.semh[e], self.cnt[e], e)
        self.last[e] = tok
        return tok

    def _deps(self, reads, writes, gid=None):
        waits = []
        for b in reads:
            if b.w is not None:
                waits.append(b.w)
        for b in writes:
            if b.w is not None and not (gid is not None and b.w[2] == ("dma", gid)):
                waits.append(b.w)
            waits.extend(b.r.values())
        return waits

    def _record(self, tok, reads, writes):
        key = (tok[0].num, tok[2])
        for b in reads:
            b.r[key] = tok
        for b in writes:
            b.w = tok
            b.r = {}

    def op(self, eng, fn, reads=(), writes=()):
        waits = self._deps(reads, writes)
        if self.pend_bar[eng]:
            waits.extend(self.pend_bar[eng])
            self.pend_bar[eng] = []
        tok = self._tick(eng)
        self.ops[eng].append((waits, fn, tok, 1))
        self._record(tok, reads, writes)
        self.nops += 1
        return tok

    def dma(self, q, fn, reads=(), writes=(), gid=None, exempt=False):
        b0 = writes[0]
        waits = self._deps(reads, writes, gid)
        if self.pend_bar[q] and not exempt:
            waits.extend(self.pend_bar[q])
            self.pend_bar[q] = []
        if b0.sem is None or b0.cnt >= 30000:
            b0.sem = self._alloc_sem("d" + b0.name)
            b0.cnt = 0
        b0.cnt += 16
        tok = (b0.sem, b0.cnt, ("dma", gid))
        self.ops[q].append((waits, fn, tok, 16))
        self._record(tok, reads, writes)
        if not exempt:
            self.dma_pending.append(tok)
        self.nops += 1
        return tok

    def barrier(self):
        toks = [self.last[e] for e in ("pe", "act", "dve", "pool") if e in self.last]
        toks.extend(self.dma_pending)
        self.dma_pending = []
        for e in self.ENGS:
            self.pend_bar[e] = list(self.pend_bar[e]) + toks

    def emit(self, final_waits):
        nc = self.nc
        engmap = {"pe": "tensor", "act": "scalar", "dve": "vector", "pool": "gpsimd", "sp": "sync"}
        with nc.Block() as block:
            for e in self.ENGS:
                def body(eng, e=e):
                    waited = {}
                    for waits, fn, tok, inc in self.ops[e]:
                        need = {}
                        for (sh, v, pe) in waits:
                            if pe == "pe" and e == "pe":
                                continue
                            k = sh.num
                            if waited.get(k, 0) >= v:
                                continue
                            if k not in need or need[k][1] < v:
                                need[k] = (sh, v)
                        for k, (sh, v) in need.items():
                            eng.wait_ge(sh, v)
                            waited[k] = v
                        ins = fn(eng)
                        ins.then_inc(tok[0], inc)
                    if e == "sp":
                        for (sh, v, _) in final_waits:
                            if waited.get(sh.num, 0) < v:
                                eng.wait_ge(sh, v)
                                waited[sh.num] = v
                getattr(block, engmap[e])(body)


def build_program(n_layers=DEPTH, debug_raw=False):
    nc = bass.Bass("TRN2", target_bir_lowering=False)
    P = Prog(nc)

    def din(name, shape, dt=F32):
        return nc.dram_tensor(name, list(shape), dt, kind="ExternalInput").ap()

    x_in = din("x", [TOK, D])
    cT_in = din("cT", [128, NCH, NB_CORE])
    vecs_in = din("vecs", [128, NV])
    consts_in = din("consts", [128, NCONST])
    wr_in = din("wr", [128, DEPTH, NCH, 20])
    rb_in = din("rb", [1, DEPTH * 20])
    lam_in = din("lamv", [128, 2 * 4 * 64])
    subg_in = din("subgT", [128, 2])
    mod_w = din("mod_w", [DEPTH, D, 6 * D])
    kv_mod_w = din("kv_mod_w", [D, 2 * D])
    conv_in_w = din("conv_in_w", [N_A, D, 3 * D])
    conv_out_w = din("conv_out_w", [N_A, D, D])
    kv_w = din("kv_w", [D, 2 * D])
    q_w = din("q_w", [2, D, D])
    o_w = din("o_w", [2, D, D])
    exp_w1 = din("exp_w1", [DEPTH, NE, D, DE])
    exp_w3 = din("exp_w3", [DEPTH, NE, D, DE])
    exp_w2 = din("exp_w2", [DEPTH, NE, DE, D])
    out = nc.dram_tensor("out", [TOK, D], F32, kind="ExternalOutput").ap()

    xT_d = nc.dram_tensor("xT_d", [128, NCH, TOK], F32).ap()
    kT_d = nc.dram_tensor("kT_d", [128, NH, TOK], BF16).ap()
    v_d = nc.dram_tensor("v_d", [TOK, D], BF16).ap()
    xd_bufs = [Buf(f"xd{i}") for i in range(NST)]
    kd_bufs = [Buf(f"kd{i}") for i in range(NST)]
    vd_bufs = kd_bufs
    out_bufs = [Buf(f"od{i}") for i in range(NST)]

    cur = [16512]

    def salloc(name, shape, dt, at=None):
        nbytes = int(np.prod(shape[1:])) * (4 if dt == F32 else 2)
        if at is None:
            off = cur[0]
            cur[0] += (nbytes + 31) // 32 * 32
        else:
            off = at
        assert off + nbytes <= 229344, (name, off, nbytes)
        return nc.alloc_sbuf_tensor_at(name, list(shape), dt, offset=off)

    consts = salloc("consts", [128, NCONST], F32)
    ident_b = salloc("ident_b", [128, 128], BF16)
    ones_b = salloc("ones_b", [128, 128], BF16)
    ones_f = salloc("ones_f", [128, 128], F32)
    vecs = salloc("vecs", [128, NV], F32)
    wr_s = salloc("wr_s", [128, DEPTH, NCH, 20], F32)
    rb_s = salloc("rb_s", [1, DEPTH * 20], F32)
    cact = salloc("cact", [128, NCH, NB_CORE], F32)
    modT = salloc("modT", [128, 4 * 48 + 16, NB_CORE], F32)
    gm = salloc("gm", [128, 9, NCH, NB_CORE], F32)
    neglam = salloc("neglam", [128, 2], F32)
    subg = salloc("subgT", [128, 2], F32)
    zhist = salloc("zhist", [128, N_A * NCH, 2], F32)
    rstd = salloc("rstd", [128, TS], F32)
    xT = salloc("xT", [128, NCH, TS], F32)
    hT = salloc("hT", [128, NCH, TS], BF16)
    ring_base = cur[0]
    cur[0] += 6 * 8192
    eps_t = salloc("eps_t", [128, 1], F32)
    tmp32 = salloc("tmp32", [128, 2, 512], F32)
    bias_sb = salloc("bias_sb", [128, 256], F32)
    U0 = cur[0]
    assert U0 + 76 * 1024 <= 229344, U0

    B_consts, B_identb, B_onesb, B_onesf, B_vecs, B_wr, B_rb = (Buf(n) for n in
                                                                 ("consts", "identb", "onesb", "onesf", "vecs", "wr", "rb"))
    B_cact, B_modT, B_gm, B_neglam, B_subg = (Buf(n) for n in ("cact", "modT", "gm", "neglam", "subg"))
    B_zh = [Buf(f"zh{j}") for j in range(N_A * NCH)]
    B_rstd = [Buf("rstd0"), Buf("rstd1")]
    B_xT = [[Buf(f"xT{c}_{h}") for h in range(2)] for c in range(NCH)]
    B_hT = [[Buf(f"hT{c}_{h}") for h in range(2)] for c in range(NCH)]
    B_ring = [Buf(f"ring{i}") for i in range(6)]

    def flat(bb):
        return [b for row in bb for b in row]

    psall = nc.alloc_psum_tensor("psall", [128, 8, 512], F32)

    class BankV:
        def __init__(self, i):
            self.i = i

        def __getitem__(self, idx):
            if not isinstance(idx, tuple):
                idx = (idx,)
            return psall[(idx[0], self.i) + tuple(idx[1:])]

    banks = [BankV(i) for i in range(8)]
    B_bank = [Buf(f"bank{i}") for i in range(8)]

    ident_f = consts[:, C_ID:C_ID + 128]
    Dt = consts[:, C_D:C_D + 512]
    Dm = consts[:, C_DM:C_DM + 512]

    def mm(out_ap, lhsT, rhs, start, stop, reads, writes):
        return P.op("pe", lambda e: e.matmul(out_ap, lhsT, rhs, start=start, stop=stop), reads, writes)

    def tr(out_ap, in_ap, ident, reads, writes):
        return P.op("pe", lambda e: e.transpose(out_ap, in_ap, ident), reads, writes)

    def act(out_ap, in_ap, func, reads, writes, bias=None, scale=None, accum_out=None):
        kw = {}
        if bias is not None:
            kw["bias"] = bias
        if scale is not None:
            kw["scale"] = scale
        if accum_out is not None:
            kw["accum_out"] = accum_out
        return P.op("act", lambda e: e.activation(out_ap, in_ap, func, **kw), reads, writes)

    def tt(eng, out_ap, in0, in1, op, reads, writes):
        return P.op(eng, lambda e: e.tensor_tensor(out_ap, in0, in1, op), reads, writes)

    def ts(eng, out_ap, in0, s1, s2, op0, op1, reads, writes):
        if s2 is None:
            return P.op(eng, lambda e: e.tensor_scalar(out_ap, in0, s1, None, op0), reads, writes)
        return P.op(eng, lambda e: e.tensor_scalar(out_ap, in0, s1, s2, op0, op1), reads, writes)

    def stt(out_ap, in0, scalar, in1, op0, op1, reads, writes):
        return P.op("dve", lambda e: e.scalar_tensor_tensor(out_ap, in0, scalar, in1, op0, op1), reads, writes)

    def cp(eng, out_ap, in_ap, reads, writes):
        if eng == "act":
            return P.op("act", lambda e: e.activation(out_ap, in_ap, AF.Copy), reads, writes)
        return P.op(eng, lambda e: e.tensor_copy(out_ap, in_ap), reads, writes)

    def red(out_ap, in_ap, op, reads, writes):
        return P.op("dve", lambda e: e.tensor_reduce(out_ap, in_ap, AX.X, op), reads, writes)

    def recip(out_ap, in_ap, reads, writes):
        return P.op("dve", lambda e: e.reciprocal(out_ap, in_ap), reads, writes)

    def dma(q, out_ap, in_ap, reads, writes, gid=None, exempt=False, noncontig=False):
        if noncontig:
            return P.dma(q, lambda e: e.dma_start(out=out_ap, in_=in_ap, allow_slow_non_contiguous=True), reads, writes, gid, exempt)
        return P.dma(q, lambda e: e.dma_start(out=out_ap, in_=in_ap), reads, writes, gid, exempt)

    BUFS = {}

    def GB(name):
        if name not in BUFS:
            BUFS[name] = Buf(name)
        return BUFS[name]

    gidc = [0]

    def newgid():
        gidc[0] += 1
        return gidc[0]

    dma("sp", consts[:], consts_in[:, :], [], [B_consts])
    dma("sp", vecs[:], vecs_in[:, :], [], [B_vecs])
    dma("sp", wr_s[:], wr_in[:, :, :, :], [], [B_wr])
    dma("sp", rb_s[:], rb_in[:, :], [], [B_rb])
    dma("sp", cact[:], cT_in[:, :, :], [], [B_cact])
    dma("sp", subg[:], subg_in[:, :], [], [B_subg])
    cp("dve", ident_b[:], ident_f, [B_consts], [B_identb])
    P.op("dve", lambda e: e.memset(ones_b[:], 1.0), [], [B_onesb])
    P.op("dve", lambda e: e.memset(ones_f[:], 1.0), [], [B_onesf])
    for j in range(N_A * NCH):
        P.op("dve", lambda e, j=j: e.memset(zhist[:, j, :], 0.0), [], [B_zh[j]])
    act(cact[:], cact[:], AF.Silu, [B_cact], [B_cact])

    lam_s = salloc("lam_s", [128, 2, 4, 64], F32, at=U0)
    lam_t = salloc("lam_t", [128, 2, 2, 64], F32, at=U0 + 2048)
    lam_r = salloc("lam_r", [128, 2, 2], F32, at=U0 + 3072)
    B_lam = Buf("lam")
    dma("sp", lam_s[:], lam_in[:, :].rearrange("p (l k d) -> p l k d", l=2, k=4), [], [B_lam])
    for j in range(2):
        tt("dve", lam_t[:, j, 0, :], lam_s[:, j, 0, :], lam_s[:, j, 1, :], ALU.mult, [B_lam], [B_lam])
        tt("dve", lam_t[:, j, 1, :], lam_s[:, j, 2, :], lam_s[:, j, 3, :], ALU.mult, [B_lam], [B_lam])
        red(lam_r[:, j, :], lam_t[:, j, :, :], ALU.add, [B_lam], [B_lam])
        act(lam_r[:, j, :], lam_r[:, j, :], AF.Exp, [B_lam], [B_lam])
        lam_init = 0.8 - 0.6 * float(np.exp(-0.3 * (j + N_A)))
        stt(neglam[:, j:j + 1], lam_r[:, j, 1:2], -lam_init, lam_r[:, j, 0:1], ALU.add, ALU.subtract,
            [B_lam], [B_neglam])

    stage = [salloc(f"stage{i}", [128, NCH, 512], F32, at=U0 + 8192 + i * 16384) for i in range(2)]
    B_stage = [Buf("stage0"), Buf("stage1")]
    pieces = []
    for l in range(DEPTH):
        for pc in range(12):
            pieces.append((mod_w[l], pc, l * 48 + pc * 4, V_MODB + l * 48 + pc * 4))
    for pc in range(4):
        pieces.append((kv_mod_w, pc, 192 + pc * 4, V_KVB + pc * 4))
    for i, (src, pc, col0, vcol0) in enumerate(pieces):
        sb, st_ = B_stage[i % 2], stage[i % 2]
        dma("sp", st_[:], src[:, pc * 512:(pc + 1) * 512].rearrange("(kc p) n -> p kc n", p=128), [], [sb])
        bk = i % 2
        for jj in range(4):
            for kc in range(NCH):
                mm(banks[bk][:, jj * 2:jj * 2 + 2], st_[:, kc, jj * 128:(jj + 1) * 128], cact[:, kc, :],
                   kc == 0, kc == NCH - 1, [sb, B_cact], [B_bank[bk]])
        for jj in range(4):
            ts("dve", modT[:, col0 + jj, :], banks[bk][:, jj * 2:jj * 2 + 2], vecs[:, vcol0 + jj:vcol0 + jj + 1], None,
               ALU.add, None, [B_bank[bk], B_vecs], [B_modT])

    def mcol(l, which):
        return l * 48 + which * 8

    for l in range(DEPTH):
        for (slot, which, vg) in ((l, 1, V_GMIX + l * 8), (4 + l, 4, V_GFFN + l * 8)):
            for b in range(NB_CORE):
                stt(gm[:, slot, :, b], modT[:, mcol(l, which):mcol(l, which) + 8, b], 1.0, vecs[:, vg:vg + 8],
                    ALU.add, ALU.mult, [B_modT, B_vecs], [B_gm])
    for b in range(NB_CORE):
        stt(gm[:, 8, :, b], modT[:, 192 + 8:192 + 16, b], 1.0, vecs[:, V_GKV:V_GKV + 8], ALU.add, ALU.mult,
            [B_modT, B_vecs], [B_gm])
    bias_vals = {}
    B_bias = Buf("bias")
    for hd_ in range(NH):
        for kk in range(0, 32):
            v_ = float(np.float32(-SLOPES[hd_] * 128.0 * kk))
            if v_ not in bias_vals:
                k_ = len(bias_vals)
                assert k_ < 256
                bias_vals[v_] = k_
                P.op("dve", lambda e, k_=k_, v_=v_: e.memset(bias_sb[:, k_:k_ + 1], v_), [], [B_bias])
    P.barrier()

    def norm_stats():
        for h in range(2):
            for c in range(NCH):
                act(hT[:, c, h * 512:(h + 1) * 512], xT[:, c, h * 512:(h + 1) * 512], AF.Square,
                    [B_xT[c][h]], [B_hT[c][h]])
        for h in range(2):
            for c in range(NCH):
                mm(banks[6 + h][:, :], ones_b[:, :], hT[:, c, h * 512:(h + 1) * 512], c == 0, c == NCH - 1,
                   [B_onesb, B_hT[c][h]], [B_bank[6 + h]])
        for h in range(2):
            act(rstd[:, h * 512:(h + 1) * 512], banks[6 + h][:, :], AF.Sqrt, [B_bank[6 + h]], [B_rstd[h]],
                bias=eps_ap, scale=1.0 / D)
            recip(rstd[:, h * 512:(h + 1) * 512], rstd[:, h * 512:(h + 1) * 512], [B_rstd[h]], [B_rstd[h]])

    B_eps = Buf("eps")
    P.op("dve", lambda e: e.memset(eps_t[:], EPS), [], [B_eps])
    eps_ap = eps_t[:, 0:1]

    def norm_mod(gslot, shcol, b, h32=None, B_h32=None, bar_after_stats=False):
        norm_stats()
        if bar_after_stats:
            P.barrier()
        k = 0
        for h in range(2):
            for c in range(NCH):
                sl = slice(h * 512, (h + 1) * 512)
                if h32 is None:
                    ti = k % 2
                    k += 1
                    stt(tmp32[:, ti, :], xT[:, c, sl], gm[:, gslot, c, b:b + 1], rstd[:, sl], ALU.mult,
                        ALU.mult, [B_xT[c][h], B_gm, B_rstd[h], B_eps], [B_tmp32[ti]])
                    act(hT[:, c, sl], tmp32[:, ti, :], AF.Identity,
                        [B_tmp32[ti], B_modT], [B_hT[c][h]],
                        bias=modT[:, shcol + c, b:b + 1], scale=1.0)
                else:
                    stt(h32[:, c, sl], xT[:, c, sl], gm[:, gslot, c, b:b + 1], rstd[:, sl], ALU.mult, ALU.mult,
                        [B_xT[c][h], B_gm, B_rstd[h], B_eps], [B_h32[c][h]])
                    act(h32[:, c, sl], h32[:, c, sl], AF.Identity, [B_h32[c][h], B_modT], [B_h32[c][h]],
                        bias=modT[:, shcol + c, b:b + 1], scale=1.0)
                    cp("dve" if (c + h) % 2 else "act", hT[:, c, sl], h32[:, c, sl], [B_h32[c][h]], [B_hT[c][h]])

    B_tmp32 = [Buf("tmp32a"), Buf("tmp32b")]

    def load_x(l, st):
        if l == 0:
            xtok = [salloc(f"xtok{i}_{st}", [128, D], F32, at=U0 + i * 4096) for i in range(2)]
            B_xtok = [GB("xtok0"), GB("xtok1")]
            for t8 in range(8):
                i = t8 % 2
                r0 = st * TS + t8 * 128
                dma("sp", xtok[i][:], x_in[r0:r0 + 128, :], [], [B_xtok[i]])
                for c4 in range(2):
                    bk = (t8 * 2 + c4) % 2
                    for cc in range(4):
                        c = c4 * 4 + cc
                        tr(banks[bk][:, cc * 128:(cc + 1) * 128], xtok[i][:, c * 128:(c + 1) * 128], ident_f,
                           [B_xtok[i], B_consts], [B_bank[bk]])
                    h = t8 // 4
                    cp("act" if c4 else "dve", xT[:, c4 * 4:c4 * 4 + 4, t8 * 128:(t8 + 1) * 128],
                       banks[bk][:, :].rearrange("p (c t) -> p c t", c=4), [B_bank[bk]],
                       [B_xT[c4 * 4 + cc][h] for cc in range(4)])
        else:
            g = newgid()
            for c in range(NCH):
                dma("sp", xT[:, c, :], xT_d[:, c, st * TS:(st + 1) * TS], [xd_bufs[st]], [B_xT[c][0], B_xT[c][1]], gid=g)

    def store_x(st):
        g = newgid()
        for c in range(NCH):
            dma("sp", xT_d[:, c, st * TS:(st + 1) * TS], xT[:, c, :], [B_xT[c][0], B_xT[c][1]], [xd_bufs[st]], gid=g)


    vcnt = [0]

    def rview(i, shape):
        vcnt[0] += 1
        assert int(np.prod(shape[1:])) * 2 + (i * 8192) <= 6 * 8192
        return nc.alloc_sbuf_tensor_at(f"rv{vcnt[0]}", list(shape), BF16, offset=ring_base + i * 8192)

    def uview(name, shape, dt, off):
        vcnt[0] += 1
        nbytes = int(np.prod(shape[1:])) * (4 if dt == F32 else 2)
        assert U0 + off + nbytes <= 229344, (name, off, nbytes, U0)
        return nc.alloc_sbuf_tensor_at(f"{name}_{vcnt[0]}", list(shape), dt, offset=U0 + off)

    def wload(pieces_idx, dst_ap, src_ap):
        return dma("pool", dst_ap, src_ap, [], [B_ring[i] for i in pieces_idx], exempt=True)

    def conv_mixer(l, st, b):
        first = (st % 4 == 0)
        zb = uview("zb", [128, NCH, TS], BF16, 0)
        z = uview("z", [128, TS + 2], F32, 16384)
        zc = uview("zc", [128, TS], F32, 16384 + 4128)
        cs = uview("cs", [128, 2, 512], F32, 16384 + 4128 + 4096)
        B_zb = [[GB(f"zb{c}_{h}") for h in range(2)] for c in range(NCH)]
        B_z, B_zc, B_cs = GB("z"), GB("zc"), [GB("cs0"), GB("cs1")]
        norm_mod(l, mcol(l, 0), b)
        P.barrier()
        wsrc = conv_in_w[l].rearrange("(kc p) (g n) -> p kc g n", p=128, g=3)
        wout = rview(4, [128, NCH, D])
        wload([4, 5], wout[:], conv_out_w[l].rearrange("(kc p) n -> p kc n", p=128))
        for j in range(NCH):
            pi = j % 2
            wj = rview(pi, [128, NCH, 3, 128])
            g_ = newgid()
            for g3 in range(3):
                dma("pool", wj[:, :, g3, :], wsrc[:, :, g3, j * 128:(j + 1) * 128], [], [B_ring[pi]], gid=g_, exempt=True)
            if first:
                P.op("dve", lambda e: e.memset(z[:, 0:2], 0.0), [], [B_z])
            else:
                cp("dve", z[:, 0:2], zhist[:, l * NCH + j, :], [B_zh[l * NCH + j]], [B_z])
            for h in range(2):
                sl = slice(h * 512, (h + 1) * 512)
                bb = 3 * h
                for g in (1, 2):
                    for kc in range(NCH):
                        mm(banks[bb + g][:, :], wj[:, kc, g, :], hT[:, kc, sl], kc == 0, kc == NCH - 1,
                           [B_ring[pi], B_hT[kc][h]], [B_bank[bb + g]])
                cp("act", cs[:, h, :], banks[bb + 1][:, :], [B_bank[bb + 1]], [B_cs[h]])
                tt("dve", z[:, 2 + h * 512:2 + (h + 1) * 512], cs[:, h, :], banks[bb + 2][:, :], ALU.mult,
                   [B_cs[h], B_bank[bb + 2]], [B_z])
            for h in range(2):
                sl = slice(h * 512, (h + 1) * 512)
                bb = 3 * h
                for kc in range(NCH):
                    mm(banks[bb][:, :], wj[:, kc, 0, :], hT[:, kc, sl], kc == 0, kc == NCH - 1,
                       [B_ring[pi], B_hT[kc][h]], [B_bank[bb]])
            cw = V_CW + (l * 8 + j) * 3
            ts("dve", zc[:, :], z[:, 2:2 + TS], vecs[:, cw + 2:cw + 3], vecs[:, V_CB + l * 8 + j:V_CB + l * 8 + j + 1],
               ALU.mult, ALU.add, [B_z, B_vecs], [B_zc])
            stt(zc[:, :], z[:, 1:1 + TS], vecs[:, cw + 1:cw + 2], zc[:, :], ALU.mult, ALU.add, [B_z, B_vecs, B_zc], [B_zc])
            stt(zc[:, :], z[:, 0:TS], vecs[:, cw:cw + 1], zc[:, :], ALU.mult, ALU.add, [B_z, B_vecs, B_zc], [B_zc])
            cp("dve", zhist[:, l * NCH + j, :], z[:, TS:TS + 2], [B_z], [B_zh[l * NCH + j]])
            for h in range(2):
                sl = slice(h * 512, (h + 1) * 512)
                tt("dve", zb[:, j, sl], zc[:, sl], banks[3 * h][:, :], ALU.mult, [B_zc, B_bank[3 * h]], [B_zb[j][h]])
        gcol = mcol(l, 2)
        for oc in range(NCH):
            for h in range(2):
                sl = slice(h * 512, (h + 1) * 512)
                bk = 6 + (oc * 2 + h) % 2
                for kc in range(NCH):
                    mm(banks[bk][:, :], wout[:, kc, oc * 128:(oc + 1) * 128], zb[:, kc, sl], kc == 0, kc == NCH - 1,
                       [B_ring[4], B_ring[5], B_zb[kc][h]], [B_bank[bk]])
                stt(xT[:, oc, sl], banks[bk][:, :], modT[:, gcol + oc, b:b + 1], xT[:, oc, sl], ALU.mult, ALU.add,
                    [B_bank[bk], B_modT, B_xT[oc][h]], [B_xT[oc][h]])

    def moe(l, st, b):
        h32 = uview("h32", [128, NCH, TS], F32, 0)
        acc = uview("acc", [128, NCH, TS], F32, 32768)
        hid = uview("hid", [128, 2, 4, 512], BF16, 65536)
        sa = uview("sa", [128, 2, 512], F32, 65536 + 8192)
        o = 65536 + 8192 + 4096
        Ls = uview("Ls", [128, 8, 20], F32, o); o += 640
        r4 = [uview(f"r4{i}", [128, 8, 4], F32, o + i * 128) for i in range(8)]; o += 8 * 128
        r1 = [uview(f"r1{i}", [128, 8], F32, o + i * 32) for i in range(8)]; o += 8 * 32
        t16 = uview("t16", [128, 8, 4, 4], F32, o); o += 512
        comb = uview("comb", [128, 8, 4, 4], F32, o); o += 512
        B_h32 = [[GB(f"h32_{c}_{h}") for h in range(2)] for c in range(NCH)]
        B_acc = [[GB(f"acc_{c}_{h}") for h in range(2)] for c in range(NCH)]
        B_hid = [[GB(f"hid_{h}_{m}") for m in range(4)] for h in range(2)]
        B_sa = [GB("sa0"), GB("sa1")]
        B_r = GB("router")
        norm_mod(4 + l, mcol(l, 3), b, h32, B_h32, bar_after_stats=True)
        LB = banks[7][:, 0:256].rearrange("p (t n) -> p t n", t=8)
        for t8 in range(8):
            h = t8 // 4
            for kc in range(NCH):
                mm(LB[:, t8, 0:20], h32[:, kc, t8 * 128:(t8 + 1) * 128], wr_s[:, l, kc, :], kc == 0, False,
                   [B_h32[kc][h], B_wr], [B_bank[7]])
            mm(LB[:, t8, 0:20], ones_f[0:1, :], rb_s[0:1, l * 20:(l + 1) * 20], False, True, [B_onesf, B_rb], [B_bank[7]])
        cp("dve", Ls[:, :, :], LB[:, :, 0:20], [B_bank[7]], [B_r])
        R = [B_r]
        gl = Ls[:, :, 0:4]
        gmax, gs, pg, m1, m2, w1, w2 = r1[0], r1[1], r1[2], r1[3], r1[4], r1[5], r1[6]
        gsh, oh, esel, d1, mk1, e2, mk2, wexp = r4

        def bc4(a):
            return a[:, :].unsqueeze(2).to_broadcast([128, 8, 4])

        red(gmax[:, :], gl, ALU.max, R, R)
        tt("dve", gsh[:, :, :], gl, bc4(gmax), ALU.subtract, R, R)
        ts("dve", oh[:, :, :], gsh[:, :, :], 0.0, None, ALU.is_equal, None, R, R)
        act(gsh[:, :, :], gsh[:, :, :], AF.Exp, R, R)
        red(gs[:, :], gsh[:, :, :], ALU.add, R, R)
        recip(pg[:, :], gs[:, :], R, R)
        el = Ls[:, :, 4:20].rearrange("p t (g e) -> p t g e", g=4)
        tt("dve", t16[:, :, :, :], el, oh[:, :, :].unsqueeze(3).to_broadcast([128, 8, 4, 4]), ALU.mult, R, R)
        tt("dve", esel[:, :, :], t16[:, :, 0, :], t16[:, :, 1, :], ALU.add, R, R)
        tt("dve", esel[:, :, :], esel[:, :, :], t16[:, :, 2, :], ALU.add, R, R)
        tt("dve", esel[:, :, :], esel[:, :, :], t16[:, :, 3, :], ALU.add, R, R)
        red(m1[:, :], esel[:, :, :], ALU.max, R, R)
        tt("dve", d1[:, :, :], esel[:, :, :], bc4(m1), ALU.subtract, R, R)
        ts("dve", mk1[:, :, :], d1[:, :, :], 0.0, None, ALU.is_equal, None, R, R)
        stt(e2[:, :, :], mk1[:, :, :], -1e30, d1[:, :, :], ALU.mult, ALU.add, R, R)
        red(m2[:, :], e2[:, :, :], ALU.max, R, R)
        tt("dve", d1[:, :, :], e2[:, :, :], bc4(m2), ALU.subtract, R, R)
        ts("dve", mk2[:, :, :], d1[:, :, :], 0.0, None, ALU.is_equal, None, R, R)
        act(w2[:, :], m2[:, :], AF.Exp, R, R)
        ts("dve", w1[:, :], w2[:, :], 1.0, None, ALU.add, None, R, R)
        recip(w1[:, :], w1[:, :], R, R)
        tt("dve", w2[:, :], w2[:, :], w1[:, :], ALU.mult, R, R)
        tt("dve", w1[:, :], w1[:, :], pg[:, :], ALU.mult, R, R)
        tt("dve", w2[:, :], w2[:, :], pg[:, :], ALU.mult, R, R)
        tt("dve", wexp[:, :, :], mk1[:, :, :], bc4(w1), ALU.mult, R, R)
        tt("dve", mk2[:, :, :], mk2[:, :, :], bc4(w2), ALU.mult, R, R)
        tt("dve", wexp[:, :, :], wexp[:, :, :], mk2[:, :, :], ALU.add, R, R)
        tt("dve", comb[:, :, :, :], oh[:, :, :].unsqueeze(3).to_broadcast([128, 8, 4, 4]),
           wexp[:, :, :].unsqueeze(2).to_broadcast([128, 8, 4, 4]), ALU.mult, R, R)
        combf = comb[:, :, :, :].rearrange("p t g e -> p t (g e)")

        wviews = {}

        def S1(u, e, h):
            s3 = 3 * (e % 2)
            if h == 0:
                w1v = rview(s3, [128, NCH, DE])
                w3v = rview(s3 + 1, [128, NCH, DE])
                w2v = rview(s3 + 2, [128, 4, D])
                wload([s3], w1v[:], exp_w1[l, e].rearrange("(kc p) n -> p kc n", p=128))
                wload([s3 + 1], w3v[:], exp_w3[l, e].rearrange("(kc p) n -> p kc n", p=128))
                wload([s3 + 2], w2v[:], exp_w2[l, e].rearrange("(kc p) n -> p kc n", p=128))
                wviews[e] = (w1v, w3v, w2v)
            w1v, w3v, w2v = wviews[e]
            sl = slice(h * 512, (h + 1) * 512)
            bbk = 4 if u % 2 == 0 else 7

            def hid_op(m):
                tt("dve", hid[:, h, m, :], sa[:, m % 2, :], banks[bbk][:, :], ALU.mult, [B_sa[m % 2], B_bank[bbk]],
                   [B_hid[h][m]])

            for m in range(4):
                ba, bb_ = 2 * (m % 2), 2 * (m % 2) + 1
                for kc in range(NCH):
                    mm(banks[ba][:, :], w1v[:, kc, m * 128:(m + 1) * 128], hT[:, kc, sl], kc == 0, kc == NCH - 1,
                       [B_ring[s3], B_hT[kc][h]], [B_bank[ba]])
                for kc in range(NCH):
                    mm(banks[bb_][:, :], w3v[:, kc, m * 128:(m + 1) * 128], hT[:, kc, sl], kc == 0, kc == NCH - 1,
                       [B_ring[s3 + 1], B_hT[kc][h]], [B_bank[bb_]])
                if m == 1:
                    for t4 in range(4):
                        t8 = h * 4 + t4
                        mm(banks[bbk][:, t4 * 128:(t4 + 1) * 128], combf[:, t8, e:e + 1].to_broadcast([128, 128]),
                           ident_f, True, True, [B_r, B_consts], [B_bank[bbk]])
                    hid_op(0)
                act(sa[:, m % 2, :], banks[ba][:, :], AF.Silu, [B_bank[ba]], [B_sa[m % 2]])
                tt("dve", sa[:, m % 2, :], sa[:, m % 2, :], banks[bb_][:, :], ALU.mult, [B_sa[m % 2], B_bank[bb_]],
                   [B_sa[m % 2]])
                if m >= 1:
                    hid_op(m)

        def S2(u, e, h):
            s3 = 3 * (e % 2)
            w1v, w3v, w2v = wviews[e]
            sl = slice(h * 512, (h + 1) * 512)
            for j in range(NCH):
                bo = 5 + (j % 2)
                for m in range(4):
                    mm(banks[bo][:, :], w2v[:, m, j * 128:(j + 1) * 128], hid[:, h, m, :], m == 0, m == 3,
                       [B_ring[s3 + 2], B_hid[h][m]], [B_bank[bo]])
                if e == 0:
                    cp("act", acc[:, j, sl], banks[bo][:, :], [B_bank[bo]], [B_acc[j][h]])
                else:
                    tt("dve", acc[:, j, sl], acc[:, j, sl], banks[bo][:, :], ALU.add, [B_acc[j][h], B_bank[bo]],
                       [B_acc[j][h]])

        units = [(e, h) for e in range(NE) for h in range(2)]
        for u, (e, h) in enumerate(units):
            S1(u, e, h)
            if u >= 1:
                S2(u - 1, *units[u - 1])
        S2(len(units) - 1, *units[-1])
        gcol = mcol(l, 5)
        for j in range(NCH):
            for h in range(2):
                sl = slice(h * 512, (h + 1) * 512)
                stt(xT[:, j, sl], acc[:, j, sl], modT[:, gcol + j, b:b + 1], xT[:, j, sl], ALU.mult, ALU.add,
                    [B_acc[j][h], B_modT, B_xT[j][h]], [B_xT[j][h]])

    def kv_stage(st, b):
        kst = uview("kst", [128, NH, TS], BF16, 0)
        vst = uview("vst", [128, 8, D], BF16, 16384)
        B_kst = [GB(f"kst{i}") for i in range(NH)]
        B_vst = [GB(f"vst{i}") for i in range(8)]
        norm_mod(8, 192, b)
        wk = rview(0, [128, NCH, D])
        wv = rview(2, [128, NCH, D])
        wload([0, 1], wk[:], kv_w[:, 0:D].rearrange("(kc p) n -> p kc n", p=128))
        wload([2, 3], wv[:], kv_w[:, D:2 * D].rearrange("(kc p) n -> p kc n", p=128))
        n = 0
        for hd in range(NH):
            for h in range(2):
                sl = slice(h * 512, (h + 1) * 512)
                bk = n % 4; n += 1
                for kc in range(NCH):
                    mm(banks[bk][:, :], wk[:, kc, hd * 128:(hd + 1) * 128], hT[:, kc, sl], kc == 0, kc == NCH - 1,
                       [B_ring[0], B_ring[1], B_hT[kc][h]], [B_bank[bk]])
                cp("act" if n % 2 else "dve", kst[:, hd, sl], banks[bk][:, :], [B_bank[bk]], [B_kst[hd]])
        g = newgid()
        for hd in range(NH):
            dma("sp", kT_d[:, hd, st * TS:(st + 1) * TS], kst[:, hd, :], [B_kst[hd]], [kd_bufs[st]], gid=g)
        for t8 in range(8):
            h = t8 // 4
            for nh in range(2):
                bk = n % 4; n += 1
                for kc in range(NCH):
                    mm(banks[bk][:, :], hT[:, kc, t8 * 128:(t8 + 1) * 128], wv[:, kc, nh * 512:(nh + 1) * 512], kc == 0,
                       kc == NCH - 1, [B_ring[2], B_ring[3], B_hT[kc][h]], [B_bank[bk]])
                cp("act" if n % 2 else "dve", vst[:, t8, nh * 512:(nh + 1) * 512], banks[bk][:, :], [B_bank[bk]],
                   [B_vst[t8]])
        for t8 in range(8):
            r0 = st * TS + t8 * 128
            dma("sp", v_d[r0:r0 + 128, :], vst[:, t8, :], [B_vst[t8]], [vd_bufs[st]], gid=g)

    def attn(l, st, b):
        ja = l - N_A
        lam_init = 0.8 - 0.6 * float(np.exp(-0.3 * l))
        sq = st % 4
        s0 = (st // 4) * 4
        nkeys = (sq + 1) * TS
        nkt_all = nkeys // 128
        NPB, NTB, LOOK = 4, 3, 2
        qT = uview("qT", [128, NH, TS], BF16, 0)
        kh = [uview(f"kh{i}", [128, SEQ], BF16, 16384 + i * 8192) for i in range(2)]
        vh = [uview(f"vh{i}", [128, 32, 128], BF16, 32768 + i * 8192) for i in range(2)]
        pb = [uview(f"pb{i}", [128, 2, 512], BF16, 49152 + i * 2048) for i in range(NPB)]
        o = 49152 + NPB * 2048
        tb = [uview(f"tb{i}", [128, 2, 512], F32, o + i * 4096) for i in range(NTB)]
        o += NTB * 4096
        r1 = uview("r1", [128, 512], F32, o); o += 2048
        r2 = uview("r2", [128, 512], F32, o); o += 2048
        e1 = uview("e1", [128, 512], F32, o); o += 2048
        e2 = uview("e2", [128, 512], F32, o); o += 2048
        sqb = uview("sqb", [128, 512], BF16, o); o += 1024
        subs = uview("subs", [128, 1], F32, o); o += 32
        B_q = [[GB(f"q{a}_{h}") for h in range(2)] for a in range(NH)]
        B_kh, B_vh = [GB("kh0"), GB("kh1")], [GB("vh0"), GB("vh1")]
        B_pb, B_tb = [GB(f"pb{i}") for i in range(NPB)], [GB(f"tb{i}") for i in range(NTB)]
        B_r1, B_r2, B_e1, B_e2, B_sqb, B_subs = (GB(n) for n in ("ar1", "ar2", "ae1", "ae2", "asqb", "asubs"))
        norm_mod(l, mcol(l, 0), b)
        P.barrier()
        wq = rview(0, [128, NCH, D])
        wload([0, 1], wq[:], q_w[ja].rearrange("(kc p) n -> p kc n", p=128))
        wo = rview(2, [128, NCH, D])
        wload([2, 3], wo[:], o_w[ja].rearrange("(kc p) n -> p kc n", p=128))
        ts("dve", subs[:, :], subg[:, ja:ja + 1], 1.0 - lam_init, None, ALU.mult, None, [B_subg], [B_subs])

        def load_kv(hd):
            ki = hd % 2
            tk0 = s0 * TS
            dma("sp", kh[ki][:, 0:nkeys], kT_d[:, hd, tk0:tk0 + nkeys], kd_bufs[s0:s0 + sq + 1], [B_kh[ki]])
            dma("sp", vh[ki][:, 0:nkt_all, :],
                v_d[tk0:tk0 + nkeys, hd * 128:(hd + 1) * 128].rearrange("(kt p) d -> p kt d", p=128),
                vd_bufs[s0:s0 + sq + 1], [B_vh[ki]])

        load_kv(0)
        load_kv(1)
        n = 0
        for hd in range(NH):
            for h in range(2):
                sl = slice(h * 512, (h + 1) * 512)
                bk = n % 2; n += 1
                for kc in range(NCH):
                    mm(banks[bk][:, :], wq[:, kc, hd * 128:(hd + 1) * 128], hT[:, kc, sl], kc == 0, kc == NCH - 1,
                       [B_ring[0], B_ring[1], B_hT[kc][h]], [B_bank[bk]])
                cp("act" if n % 2 else "dve", qT[:, hd, sl], banks[bk][:, :], [B_bank[bk]], [B_q[hd][h]])
        tiles = []
        for hd in range(NH):
            for h in range(2):
                q0 = sq * TS + h * 512
                nkt = q0 // 128 + 4
                for kt in range(nkt):
                    tiles.append((hd, h, kt, q0, nkt))

        def front(n, t):
            hd, h, kt, q0, nkt = t
            ki = hd % 2
            slope = SLOPES[hd]
            jd = kt - q0 // 128
            qoff = 128 * jd if jd > 0 else 0
            nn = 512 - qoff
            sp, pi, ti = 2 * (n % 2), n % NPB, n % NTB
            for s in range(2):
                ps = slice(s * 64, (s + 1) * 64)
                mm(banks[sp + s][:, 0:nn], kh[ki][ps, kt * 128:(kt + 1) * 128],
                   qT[ps, hd, h * 512 + qoff:(h + 1) * 512], True, True, [B_kh[ki], B_q[hd][h]], [B_bank[sp + s]])
            dsel = Dm if jd >= 0 else Dt
            stt(tb[ti][:, :, 0:nn], dsel[:, 0:nn].unsqueeze(1).to_broadcast([128, 2, nn]), -slope * 8.0,
                psall[:, sp:sp + 2, 0:nn], ALU.mult, ALU.add, [B_consts, B_bank[sp], B_bank[sp + 1]], [B_tb[ti]])
            cb = -slope * float(q0 - kt * 128) if jd < 0 else 0.0
            act(pb[pi][:, :, qoff:512], tb[ti][:, :, 0:nn], AF.Exp, [B_tb[ti], B_bias], [B_pb[pi]],
                bias=bias_tile(cb), scale=0.125)

        def back(n, t):
            hd, h, kt, q0, nkt = t
            ki = hd % 2
            jd = kt - q0 // 128
            qoff = 128 * jd if jd > 0 else 0
            pi = n % NPB
            first, last = (kt == 0), (kt == nkt - 1)
            for s in range(2):
                mm(banks[4 + s][:, qoff:512], vh[ki][:, kt, :], pb[pi][:, s, qoff:512], first, last,
                   [B_vh[ki], B_pb[pi]], [B_bank[4 + s]])
            for s in range(2):
                mm(banks[6 + s][:, qoff:512], ones_b[:, :], pb[pi][:, s, qoff:512], first, last,
                   [B_onesb, B_pb[pi]], [B_bank[6 + s]])
            if not last:
                return
            cp("dve", e1[:, :], banks[4][:, :], [B_bank[4]], [B_e1])
            cp("dve", e2[:, :], banks[5][:, :], [B_bank[5]], [B_e2])
            act(r1[:, :], banks[6][:, :], AF.Ln, [B_bank[6]], [B_r1])
            act(r2[:, :], banks[7][:, :], AF.Ln, [B_bank[7]], [B_r2])
            act(r1[:, :], r1[:, :], AF.Exp, [B_r1], [B_r1], scale=-1.0)
            act(r2[:, :], r2[:, :], AF.Exp, [B_r2], [B_r2], scale=-1.0)
            tt("dve", e1[:, :], e1[:, :], r1[:, :], ALU.mult, [B_e1, B_r1], [B_e1])
            stt(e2[:, :], e2[:, :], neglam[:, ja:ja + 1], r2[:, :], ALU.mult, ALU.mult,
                [B_e2, B_neglam, B_r2], [B_e2])
            tt("pool", e1[:, :], e1[:, :], e2[:, :], ALU.add, [B_e1, B_e2], [B_e1])
            tt("pool", sqb[:, :], e1[:, :], e1[:, :], ALU.mult, [B_e1], [B_sqb])
            pending.append((n + LOOK + DELAY, hd, h))
            if h == 1 and hd + 2 < NH:
                load_kv(hd + 2)

        def evac2(hd, h, bk):
            sl = slice(h * 512, (h + 1) * 512)
            mm(banks[bk][:, :], ones_b[:, :], sqb[:, :], True, True, [B_onesb, B_sqb], [B_bank[bk]])
            act(r1[:, :], banks[bk][:, :], AF.Ln, [B_bank[bk], B_eps], [B_r1], bias=eps_ap, scale=1.0 / 128)
            act(r1[:, :], r1[:, :], AF.Exp, [B_r1], [B_r1], scale=-0.5)
            stt(hT[:, hd, sl], e1[:, :], subs[:, 0:1], r1[:, :], ALU.mult, ALU.mult, [B_e1, B_subs, B_r1],
                [B_hT[hd][h]])

        DELAY = 2
        pending = []
        for n, t in enumerate(tiles):
            front(n, t)
            if n >= LOOK:
                back(n - LOOK, tiles[n - LOOK])
            while pending and pending[0][0] <= n:
                _, hd_, h_ = pending.pop(0)
                evac2(hd_, h_, 2 * ((n + 1) % 2))
        for n in range(max(len(tiles) - LOOK, 0), len(tiles)):
            back(n, tiles[n])
        for (_, hd_, h_) in pending:
            evac2(hd_, h_, 0)
        gcol = mcol(l, 2)
        for oc in range(NCH):
            for h in range(2):
                sl = slice(h * 512, (h + 1) * 512)
                bk = (oc * 2 + h) % 2
                for kc in range(NCH):
                    mm(banks[bk][:, :], wo[:, kc, oc * 128:(oc + 1) * 128], hT[:, kc, sl], kc == 0, kc == NCH - 1,
                       [B_ring[2], B_ring[3], B_hT[kc][h]], [B_bank[bk]])
                stt(xT[:, oc, sl], banks[bk][:, :], modT[:, gcol + oc, b:b + 1], xT[:, oc, sl], ALU.mult, ALU.add,
                    [B_bank[bk], B_modT, B_xT[oc][h]], [B_xT[oc][h]])

    def bias_tile(v):
        k = bias_vals[float(np.float32(v))]
        return bias_sb[:, k:k + 1]

    def final_out(st, raw):
        y32 = uview("y32", [128, NCH, TS], F32, 0)
        otok = [uview(f"otok{i}", [128, D], F32, 32768 + i * 4096) for i in range(2)]
        B_y = [[GB(f"y{c}_{h}") for h in range(2)] for c in range(NCH)]
        B_ot = [GB("ot0"), GB("ot1")]
        if not raw:
            norm_stats()
        for c in range(NCH):
            for h in range(2):
                sl = slice(h * 512, (h + 1) * 512)
                if raw:
                    cp("dve", y32[:, c, sl], xT[:, c, sl], [B_xT[c][h]], [B_y[c][h]])
                else:
                    stt(y32[:, c, sl], xT[:, c, sl], vecs[:, V_GFIN + c:V_GFIN + c + 1], rstd[:, sl], ALU.mult, ALU.mult,
                        [B_xT[c][h], B_vecs, B_rstd[h]], [B_y[c][h]])
        g = newgid()
        n = 0
        for t8 in range(8):
            h = t8 // 4
            i = t8 % 2
            for c4 in range(2):
                bk = n % 2; n += 1
                for cc in range(4):
                    c = c4 * 4 + cc
                    tr(banks[bk][:, cc * 128:(cc + 1) * 128], y32[:, c, t8 * 128:(t8 + 1) * 128], ident_f,
                       [B_y[c][h], B_consts], [B_bank[bk]])
                cp("act" if c4 else "dve", otok[i][:, c4 * 512:(c4 + 1) * 512], banks[bk][:, :], [B_bank[bk]], [B_ot[i]])
            r0 = st * TS + t8 * 128
            dma("sp", out[r0:r0 + 128, :], otok[i][:], [B_ot[i]], [out_bufs[st]], gid=g)

    for st in range(NST):
        b = st // 4
        P.barrier()
        load_x(0, st)
        for l in range(n_layers):
            if l < N_A:
                conv_mixer(l, st, b)
            else:
                attn(l, st, b)
            moe(l, st, b)
            if l == N_A - 1 and n_layers > N_A:
                P.barrier()
                kv_stage(st, b)
        P.barrier()
        final_out(st, raw=(debug_raw or n_layers < DEPTH))
    finals = [bf.w for bf in out_bufs if bf.w is not None]
    P.emit(finals)
    return nc, P


def _fm(v):
    v = np.asarray(v, np.float32).reshape(-1, 128)
    return np.ascontiguousarray(v.T)


def _host_layout(inputs, core):
    f32 = np.float32
    b0 = core * NB_CORE
    m = {}
    m["x"] = np.ascontiguousarray(inputs["x"][b0:b0 + NB_CORE].reshape(TOK, D))
    c2 = np.asarray(inputs["c"][b0:b0 + NB_CORE], f32)
    m["cT"] = np.ascontiguousarray(c2.T.reshape(NCH, 128, NB_CORE).transpose(1, 0, 2))
    vec = np.zeros((128, NV), f32)
    vec[:, V_MODB:V_MODB + 192] = _fm(inputs["mod_b"])
    vec[:, V_KVB:V_KVB + 16] = _fm(inputs["kv_mod_b"])
    vec[:, V_GMIX:V_GMIX + 32] = _fm(inputs["norm_mix_g"])
    vec[:, V_GFFN:V_GFFN + 32] = _fm(inputs["norm_ffn_g"])
    vec[:, V_GKV:V_GKV + 8] = _fm(inputs["kv_norm_g"])
    vec[:, V_GFIN:V_GFIN + 8] = _fm(inputs["final_norm_g"])
    cw = np.asarray(inputs["conv_w"], f32)
    vec[:, V_CW:V_CW + 48] = cw.reshape(2, 3, NCH, 128).transpose(3, 0, 2, 1).reshape(128, 48)
    vec[:, V_CB:V_CB + 16] = _fm(inputs["conv_b"])
    m["vecs"] = vec
    k = np.arange(128)[:, None].astype(f32)
    q = np.arange(512)[None, :].astype(f32)
    cst = np.zeros((128, NCONST), f32)
    cst[:, C_ID:C_ID + 128] = np.eye(128, dtype=f32)
    cst[:, C_D:C_D + 512] = q - k
    dmk = (q - k).copy()
    dmk[(q - k) < 0] = 1e30
    cst[:, C_DM:C_DM + 512] = dmk
    m["consts"] = cst
    wr = np.concatenate([np.asarray(inputs["router_group_w"], f32), np.asarray(inputs["router_exp_w"], f32)], axis=-1)
    m["wr"] = np.ascontiguousarray(wr.reshape(DEPTH, NCH, 128, 20).transpose(2, 0, 1, 3))
    rb = np.concatenate([np.asarray(inputs["router_group_b"], f32), np.asarray(inputs["router_exp_b"], f32)], axis=-1)
    m["rb"] = np.ascontiguousarray(rb.reshape(1, DEPTH * 20))
    lam = np.stack([inputs["lam_q1"], inputs["lam_k1"], inputs["lam_q2"], inputs["lam_k2"]], axis=1)
    m["lamv"] = np.ascontiguousarray(np.broadcast_to(np.asarray(lam, f32).reshape(1, -1), (128, 512)))
    m["subgT"] = np.ascontiguousarray(np.asarray(inputs["subln_g"], f32).T)
    for kname in ("mod_w", "kv_mod_w", "conv_in_w", "conv_out_w", "kv_w", "q_w", "o_w", "exp_w1", "exp_w3", "exp_w2"):
        m[kname] = np.ascontiguousarray(np.asarray(inputs[kname], f32))
    return m


def kernel(**inputs):
    nc, _ = build_program()
    in_maps = [_host_layout(inputs, core) for core in range(8)]
    res = run_bass_kernel_spmd(nc, in_maps, core_ids=list(range(8)))
    outs = [np.asarray(r["out"], np.float32).reshape(NB_CORE, SEQ, D) for r in res.results]
    return np.concatenate(outs, axis=0)
```
